# Optimizing a Trainium2 kernel written in Bass

```python
import math
import jax, jax.numpy as jnp
from jax import lax
import numpy as np

D_MODEL = 1024
BATCH = 2
SEQ = 16384
DEPTH = 2

GRID_W = 64
CTX_LEN = 256
N_EVEN = (DEPTH + 1) // 2
N_ODD = DEPTH // 2
N_MOD = 6
EPS = 1e-6
Q_BLOCK = 128
ROPE_BASE = 10000.0

POOL_WINDOWS = (2, 4, 8, 16)
POOL_GROUP = D_MODEL // 16
POOL_WIDTH = POOL_GROUP * len(POOL_WINDOWS)
DIFF_HEAD_DIM = 64
DIFF_V_DIM = 2 * DIFF_HEAD_DIM
DIFF_HEADS = (D_MODEL - POOL_WIDTH) // DIFF_V_DIM
DIFF_QK_WIDTH = DIFF_HEADS * 2 * DIFF_HEAD_DIM
DIFF_V_WIDTH = DIFF_HEADS * DIFF_V_DIM
EVEN_IN_WIDTH = POOL_WIDTH + 2 * DIFF_QK_WIDTH + DIFF_V_WIDTH
EVEN_MIX_WIDTH = POOL_WIDTH + DIFF_V_WIDTH
MLA_HEADS = D_MODEL // 128
MLA_NOPE = 128
MLA_ROPE = 64
MLA_V = 128
MLA_Q_RANK = D_MODEL // 2
MLA_KV_RANK = D_MODEL // 4
ODD_IN_WIDTH = MLA_Q_RANK + MLA_KV_RANK + MLA_ROPE
MLA_MIX_WIDTH = MLA_HEADS * MLA_V
FFN_HIDDEN = ((8 * D_MODEL // 3 + 255) // 256) * 256
N_EXPERTS = 8
TOP_K = 2

kernel_name = "hybrid_pool_diffattn_mla_moe_prefix_dit"

F32 = jnp.float32


def rmsnorm(h, g=None):
    hf = h.astype(F32)
    y = hf * lax.rsqrt(jnp.mean(hf * hf, axis=-1, keepdims=True) + EPS)
    if g is not None:
        y = y * g.astype(F32)
    return y.astype(h.dtype)


def adaln(cond, w, b):
    return jax.nn.silu(cond) @ w + b


def modulate(h, shift, scale):
    return h * (1.0 + scale) + shift


def axial_rope_tables(rows, dim):
    t = jnp.arange(rows * GRID_W, dtype=jnp.int32)
    row = (t // GRID_W).astype(F32)
    col = (t % GRID_W).astype(F32)
    quarter = dim // 4
    inv = ROPE_BASE ** (-jnp.arange(quarter, dtype=F32) / quarter)
    ar = row[:, None] * inv[None, :]
    ac = col[:, None] * inv[None, :]
    cos = jnp.concatenate([jnp.cos(ar), jnp.cos(ar), jnp.cos(ac), jnp.cos(ac)], axis=-1)
    sin = jnp.concatenate([jnp.sin(ar), jnp.sin(ar), jnp.sin(ac), jnp.sin(ac)], axis=-1)
    return cos, sin


def apply_axial_rope(u, cos, sin):
    u1, u2, u3, u4 = jnp.split(u, 4, axis=-1)
    rot = jnp.concatenate([-u2, u1, -u4, u3], axis=-1)
    return (u.astype(F32) * cos + rot.astype(F32) * sin).astype(u.dtype)


def sweep_query_blocks(fn, qs):
    b, l = qs[0].shape[:2]
    nb = l // Q_BLOCK
    blocked = tuple(jnp.moveaxis(q.reshape((b, nb, Q_BLOCK) + q.shape[2:]), 1, 0) for q in qs)
    out = lax.map(lambda qb: fn(*qb), blocked)
    return jnp.moveaxis(out, 0, 1).reshape((b, l) + out.shape[3:])


def softmax_attn_block(q, k, v, scale):
    s = jnp.einsum('bqhd,bkhd->bhqk', q, k).astype(F32) * scale
    p = jax.nn.softmax(s, axis=-1)
    return jnp.einsum('bhqk,bkhe->bqhe', p.astype(v.dtype), v)


def diff_attn_block(q1, q2, k1, k2, v, lam):
    scale = DIFF_HEAD_DIM ** -0.5
    s1 = jnp.einsum('bqhd,bkhd->bhqk', q1, k1).astype(F32) * scale
    s2 = jnp.einsum('bqhd,bkhd->bhqk', q2, k2).astype(F32) * scale
    a = jax.nn.softmax(s1, axis=-1) - lam * jax.nn.softmax(s2, axis=-1)
    return jnp.einsum('bhqk,bkhe->bqhe', a.astype(v.dtype), v)


def multiscale_pool(u, pool_w, pool_scale):
    b, l, _ = u.shape
    uf = u.astype(F32)
    cs = jnp.concatenate([jnp.zeros((b, 1, POOL_WIDTH), F32), jnp.cumsum(uf, axis=1)], axis=1)
    t = jnp.arange(l)
    outs = []
    for g, w in enumerate(POOL_WINDOWS):
        lo = jnp.clip(t - w // 2, 0, l)
        hi = jnp.clip(t + w // 2, 0, l)
        sl = slice(g * POOL_GROUP, (g + 1) * POOL_GROUP)
        csg = cs[:, :, sl]
        s = jnp.take(csg, hi, axis=1) - jnp.take(csg, lo, axis=1)
        cnt = (hi - lo).astype(F32)[None, :, None]
        outs.append(s / cnt - uf[:, :, sl])
    p = jnp.stack(outs, axis=2).astype(u.dtype)
    y = jnp.einsum('blgc,gcd->blgd', p, pool_w).reshape(b, l, POOL_WIDTH)
    return y * pool_scale


def split_even(z):
    n = z.shape[:2]
    o = POOL_WIDTH
    p = z[..., :o]
    q = z[..., o:o + DIFF_QK_WIDTH].reshape(n + (DIFF_HEADS, 2, DIFF_HEAD_DIM))
    k = z[..., o + DIFF_QK_WIDTH:o + 2 * DIFF_QK_WIDTH].reshape(n + (DIFF_HEADS, 2, DIFF_HEAD_DIM))
    v = z[..., o + 2 * DIFF_QK_WIDTH:].reshape(n + (DIFF_HEADS, DIFF_V_DIM))
    return p, q, k, v


def even_merge(p, attn, pool_w, pool_scale, w_out, lam_init):
    b, l = p.shape[:2]
    pooled = multiscale_pool(p, pool_w, pool_scale)
    heads = (rmsnorm(attn) * (1.0 - lam_init)).reshape(b, l, DIFF_V_WIDTH)
    return jnp.concatenate([pooled, heads], axis=-1) @ w_out


def even_mixer(a_lat, a_ctx, w_in, pool_w, pool_scale, lq1, lk1, lq2, lk2, w_out,
               cos, sin, lam_init, need_ctx):
    p_lat, q_lat, k_lat, v_lat = split_even(a_lat @ w_in)
    p_ctx, q_ctx, k_ctx, v_ctx = split_even(a_ctx @ w_in)
    cs, sn = cos[None, :, None, None, :], sin[None, :, None, None, :]
    q_lat = apply_axial_rope(q_lat, cs, sn)
    k_lat = apply_axial_rope(k_lat, cs, sn)
    lam = (jnp.exp(jnp.sum(lq1.astype(F32) * lk1.astype(F32)))
           - jnp.exp(jnp.sum(lq2.astype(F32) * lk2.astype(F32))) + lam_init)
    k_all = jnp.concatenate([k_lat, k_ctx], axis=1)
    v_all = jnp.concatenate([v_lat, v_ctx], axis=1)
    k1, k2 = k_all[..., 0, :], k_all[..., 1, :]
    attn_lat = sweep_query_blocks(
        lambda b1, b2: diff_attn_block(b1, b2, k1, k2, v_all, lam),
        (q_lat[..., 0, :], q_lat[..., 1, :]))
    y_lat = even_merge(p_lat, attn_lat, pool_w, pool_scale, w_out, lam_init)
    y_ctx = None
    if need_ctx:
        attn_ctx = diff_attn_block(q_ctx[..., 0, :], q_ctx[..., 1, :],
                                   k_ctx[..., 0, :], k_ctx[..., 1, :], v_ctx, lam)
        y_ctx = even_merge(p_ctx, attn_ctx, pool_w, pool_scale, w_out, lam_init)
    return y_lat, y_ctx


def odd_mixer(a_lat, a_ctx, w_in, q_norm_g, kv_norm_g, w_uq, w_ukv, w_out, cos, sin, need_ctx):
    z_lat = a_lat @ w_in
    z_ctx = a_ctx @ w_in

    def queries(z):
        cq = rmsnorm(z[..., :MLA_Q_RANK], q_norm_g)
        return (cq @ w_uq).reshape(z.shape[:2] + (MLA_HEADS, MLA_NOPE + MLA_ROPE))

    def keys_values(z):
        ckv = rmsnorm(z[..., MLA_Q_RANK:MLA_Q_RANK + MLA_KV_RANK], kv_norm_g)
        kv = (ckv @ w_ukv).reshape(z.shape[:2] + (MLA_HEADS, MLA_NOPE + MLA_V))
        return kv[..., :MLA_NOPE], kv[..., MLA_NOPE:], z[..., MLA_Q_RANK + MLA_KV_RANK:]

    def full_keys(k_nope, k_rope):
        shared = jnp.broadcast_to(k_rope[:, :, None, :], k_nope.shape[:3] + (MLA_ROPE,))
        return jnp.concatenate([k_nope, shared], axis=-1)

    q_lat = queries(z_lat)
    q_lat = jnp.concatenate(
        [q_lat[..., :MLA_NOPE],
         apply_axial_rope(q_lat[..., MLA_NOPE:], cos[None, :, None, :], sin[None, :, None, :])],
        axis=-1)
    kn_lat, v_lat, kr_lat = keys_values(z_lat)
    kr_lat = apply_axial_rope(kr_lat, cos[None], sin[None])
    kn_ctx, v_ctx, kr_ctx = keys_values(z_ctx)
    k_ctx = full_keys(kn_ctx, kr_ctx)
    k_all = jnp.concatenate([full_keys(kn_lat, kr_lat), k_ctx], axis=1)
    v_all = jnp.concatenate([v_lat, v_ctx], axis=1)
    scale = (MLA_NOPE + MLA_ROPE) ** -0.5
    b, l = a_lat.shape[:2]
    o_lat = sweep_query_blocks(lambda qb: softmax_attn_block(qb, k_all, v_all, scale), (q_lat,))
    y_lat = o_lat.reshape(b, l, MLA_MIX_WIDTH) @ w_out
    y_ctx = None
    if need_ctx:
        o_ctx = softmax_attn_block(queries(z_ctx), k_ctx, v_ctx, scale)
        y_ctx = o_ctx.reshape(a_ctx.shape[:2] + (MLA_MIX_WIDTH,)) @ w_out
    return y_lat, y_ctx


def swiglu(h, w_gate, w_up, w_down):
    return (jax.nn.silu(h @ w_gate) * (h @ w_up)) @ w_down


def moe_swiglu(h, router_w, w_gate, w_up, w_down):
    b, l, d = h.shape
    tok = h.reshape(-1, d)
    n = tok.shape[0]
    probs = jax.nn.softmax((tok @ router_w).astype(F32), axis=-1)
    top_p, top_e = lax.top_k(probs, TOP_K)
    top_p = top_p / jnp.sum(top_p, axis=-1, keepdims=True)
    flat_e = top_e.reshape(-1)
    order = jnp.argsort(flat_e)
    tok_idx = order // TOP_K
    xs = tok[tok_idx]
    group_sizes = jnp.bincount(flat_e, length=N_EXPERTS).astype(jnp.int32)
    hg = lax.ragged_dot(xs, w_gate, group_sizes)
    hu = lax.ragged_dot(xs, w_up, group_sizes)
    ys = lax.ragged_dot(jax.nn.silu(hg) * hu, w_down, group_sizes)
    wts = top_p.reshape(-1)[order].astype(ys.dtype)
    out = jax.ops.segment_sum(ys * wts[:, None], tok_idx, num_segments=n)
    return out.reshape(b, l, d)


def setup_inputs(seed: int = 0) -> dict:
    key = jax.random.key(seed)
    ks = iter(jax.random.split(key, 48))
    D = D_MODEL
    F = FFN_HIDDEN

    def nrm(shape, s):
        return jax.random.normal(next(ks), shape, jnp.float32) * s

    return {
        "x": nrm((BATCH, SEQ, D), 1.0),
        "c": nrm((BATCH, D), 1.0),
        "ctx": nrm((BATCH, CTX_LEN, D), 1.0),
        "c_ctx": nrm((D,), 1.0),
        "norm1_g": 1.0 + nrm((DEPTH, D), 0.1),
        "norm2_g": 1.0 + nrm((DEPTH, D), 0.1),
        "mod_w": nrm((DEPTH, D, N_MOD * D), 0.5 * D ** -0.5),
        "mod_b": nrm((DEPTH, N_MOD * D), 0.01),
        "even_w_in": nrm((N_EVEN, D, EVEN_IN_WIDTH), D ** -0.5),
        "pool_w": nrm((N_EVEN, len(POOL_WINDOWS), POOL_GROUP, POOL_GROUP), POOL_GROUP ** -0.5),
        "pool_scale": 1.0 + nrm((N_EVEN, POOL_WIDTH), 0.1),
        "lambda_q1": nrm((N_EVEN, DIFF_HEAD_DIM), 0.1),
        "lambda_k1": nrm((N_EVEN, DIFF_HEAD_DIM), 0.1),
        "lambda_q2": nrm((N_EVEN, DIFF_HEAD_DIM), 0.1),
        "lambda_k2": nrm((N_EVEN, DIFF_HEAD_DIM), 0.1),
        "even_w_out": nrm((N_EVEN, EVEN_MIX_WIDTH, D), EVEN_MIX_WIDTH ** -0.5),
        "ffn_w_gate": nrm((N_EVEN, D, F), D ** -0.5),
        "ffn_w_up": nrm((N_EVEN, D, F), D ** -0.5),
        "ffn_w_down": nrm((N_EVEN, F, D), F ** -0.5),
        "odd_w_in": nrm((N_ODD, D, ODD_IN_WIDTH), D ** -0.5),
        "q_norm_g": 1.0 + nrm((N_ODD, MLA_Q_RANK), 0.1),
        "kv_norm_g": 1.0 + nrm((N_ODD, MLA_KV_RANK), 0.1),
        "w_uq": nrm((N_ODD, MLA_Q_RANK, MLA_HEADS * (MLA_NOPE + MLA_ROPE)), MLA_Q_RANK ** -0.5),
        "w_ukv": nrm((N_ODD, MLA_KV_RANK, MLA_HEADS * (MLA_NOPE + MLA_V)), MLA_KV_RANK ** -0.5),
        "odd_w_out": nrm((N_ODD, MLA_MIX_WIDTH, D), MLA_MIX_WIDTH ** -0.5),
        "router_w": nrm((N_ODD, D, N_EXPERTS), D ** -0.5),
        "moe_w_gate": nrm((N_ODD, N_EXPERTS, D, F), D ** -0.5),
        "moe_w_up": nrm((N_ODD, N_EXPERTS, D, F), D ** -0.5),
        "moe_w_down": nrm((N_ODD, N_EXPERTS, F, D), F ** -0.5),
        "final_norm_g": 1.0 + nrm((D,), 0.1),
    }


def reference(x, c, ctx, c_ctx, norm1_g, norm2_g, mod_w, mod_b, even_w_in, pool_w, pool_scale,
              lambda_q1, lambda_k1, lambda_q2, lambda_k2, even_w_out, ffn_w_gate, ffn_w_up,
              ffn_w_down, odd_w_in, q_norm_g, kv_norm_g, w_uq, w_ukv, odd_w_out, router_w,
              moe_w_gate, moe_w_up, moe_w_down, final_norm_g):
    rows = x.shape[1] // GRID_W
    cos_d, sin_d = axial_rope_tables(rows, DIFF_HEAD_DIM)
    cos_m, sin_m = axial_rope_tables(rows, MLA_ROPE)
    h_lat, h_ctx = x, ctx
    for i in range(DEPTH):
        need_ctx = i < DEPTH - 1
        j = i // 2
        mod_lat = adaln(c, mod_w[i], mod_b[i])[:, None, :]
        mod_ctx = adaln(c_ctx, mod_w[i], mod_b[i])[None, None, :]
        sh1, sc1, g1, sh2, sc2, g2 = jnp.split(mod_lat, N_MOD, axis=-1)
        csh1, csc1, cg1, csh2, csc2, cg2 = jnp.split(mod_ctx, N_MOD, axis=-1)
        a_lat = modulate(rmsnorm(h_lat, norm1_g[i]), sh1, sc1)
        a_ctx = modulate(rmsnorm(h_ctx, norm1_g[i]), csh1, csc1)
        if i % 2 == 0:
            lam_init = 0.8 - 0.6 * math.exp(-0.3 * i)
            y_lat, y_ctx = even_mixer(a_lat, a_ctx, even_w_in[j], pool_w[j], pool_scale[j],
                                      lambda_q1[j], lambda_k1[j], lambda_q2[j], lambda_k2[j],
                                      even_w_out[j], cos_d, sin_d, lam_init, need_ctx)
            ffn = lambda h: swiglu(h, ffn_w_gate[j], ffn_w_up[j], ffn_w_down[j])
        else:
            y_lat, y_ctx = odd_mixer(a_lat, a_ctx, odd_w_in[j], q_norm_g[j], kv_norm_g[j],
                                     w_uq[j], w_ukv[j], odd_w_out[j], cos_m, sin_m, need_ctx)
            ffn = lambda h: moe_swiglu(h, router_w[j], moe_w_gate[j], moe_w_up[j], moe_w_down[j])
        h_lat = h_lat + g1 * y_lat
        h_lat = h_lat + g2 * ffn(modulate(rmsnorm(h_lat, norm2_g[i]), sh2, sc2))
        if need_ctx:
            h_ctx = h_ctx + cg1 * y_ctx
            h_ctx = h_ctx + cg2 * ffn(modulate(rmsnorm(h_ctx, norm2_g[i]), csh2, csc2))
    return rmsnorm(h_lat, final_norm_g)
```

```python
import contextlib
import math
import numpy as np
import concourse.bass as bass
import concourse.mybir as mybir
from concourse.bass_utils import run_bass_kernel_spmd

F32 = mybir.dt.float32
BF16 = mybir.dt.bfloat16
U8 = mybir.dt.uint8
AF = mybir.ActivationFunctionType
ALU = mybir.AluOpType
AX = mybir.AxisListType

D = 1024
DC = 8
FF = 2816
FC = 22
EPS = 1e-6
NEXP = 8
GRID_W = 64
POOL_WINDOWS = (2, 4, 8, 16)
ISZ = {F32: 4, BF16: 2, U8: 1}


class Buf:
    def __init__(self, name, ap=None, multi=False):
        self.name = name
        self.ap = ap
        self.multi = multi
        self.writes = []
        self.reads = {}
        self.war = []
        self.sem = None
        self.sem_count = 0


class Eng:
    def __init__(self, name, sem):
        self.name = name
        self.sem = sem
        self.count = 0
        self.known = {}
        self.items = []


class Sched:
    ENGS = ['tensor', 'vector', 'scalar', 'gpsimd', 'sync']

    def __init__(self, nc, arena_bytes=0):
        self.nc = nc
        self.stack = contextlib.ExitStack()
        self.sems = []
        self.eng = {}
        for n in self.ENGS:
            self.eng[n] = Eng(n, self.new_sem('e_' + n))
        self.dram_bufs = {}
        self.free_sems = []
        self.live = []
        self.all_dma_bufs = []
        self.top = 0
        self.arena = None
        self.arena_bytes = arena_bytes
        if arena_bytes:
            self.arena = self.stack.enter_context(nc.sbuf_tensor("arena", [128, arena_bytes], U8))

    def new_sem(self, name):
        s = self.stack.enter_context(self.nc.semaphore(name))
        self.sems.append(s)
        return len(self.sems) - 1

    def sbuf(self, name, shape, dtype, multi=False):
        t = self.stack.enter_context(self.nc.sbuf_tensor(name, list(shape), dtype))
        return Buf(name, t, multi)

    def psum(self, name, shape, dtype):
        t = self.stack.enter_context(self.nc.psum_tensor(name, list(shape), dtype))
        return Buf(name, t)

    def alloc(self, name, shape, dtype, multi=False):
        n = 1
        for s in shape:
            n *= s
        nb = n * ISZ[dtype]
        nb_al = (nb + 63) // 64 * 64
        off = self.top
        assert off + nb_al <= self.arena_bytes, (name, off, nb_al, self.arena_bytes)
        self.top += nb_al
        ap = self.arena[:, off:off + nb]
        if dtype != U8:
            ap = ap.bitcast(dtype)
        if len(shape) == 2:
            ap = ap.rearrange("p (a b) -> p a b", a=shape[0])
        elif len(shape) == 3:
            ap = ap.rearrange("p (a b c) -> p a b c", a=shape[0], b=shape[1])
        b = Buf(name, ap, multi)
        self.live.append((off, b))
        return b

    def view(self, name, ap, multi=False):
        b = Buf(name, ap, multi)
        self.live.append((self.top, b))
        return b

    def mark(self):
        return self.top

    def release(self, mark):
        keep = []
        for off, b in self.live:
            if off >= mark:
                if b.sem is not None:
                    self.free_sems.append((b.sem, b.sem_count))
                    b.sem = None
            else:
                keep.append((off, b))
        self.live = keep
        self.top = mark

    def dram_buf(self, name, multi=True):
        if name not in self.dram_bufs:
            self.dram_bufs[name] = Buf('dram_' + name, None, multi)
        return self.dram_bufs[name]

    def _deps(self, reads, writes):
        deps = []
        for b in reads:
            deps.extend(b.writes)
        for b in writes:
            if b.reads:
                b.war = list(b.reads.values())
                b.reads = {}
                b.writes = []
            deps.extend(b.war)
            if not b.multi:
                deps.extend(b.writes)
        return deps

    def _commit(self, tok, reads, writes):
        for b in reads:
            b.reads[tok[0]] = tok
        for b in writes:
            if b.multi:
                b.writes = [t for t in b.writes if t[0] != tok[0]] + [tok]
            else:
                b.writes = [tok]

    def _waits(self, E, deps):
        best = {}
        for (s, v, clk) in deps:
            if E.name == 'tensor' and s == E.sem:
                continue
            if E.known.get(s, 0) >= v:
                continue
            best[s] = max(best.get(s, 0), v)
            E.known[s] = v
            for k, kv in clk.items():
                if E.known.get(k, 0) < kv:
                    E.known[k] = kv
        return list(best.items())

    def op(self, eng, reads, writes, emit):
        E = self.eng[eng]
        ws = self._waits(E, self._deps(reads, writes))
        E.count += 1
        tok = (E.sem, E.count, dict(E.known))
        E.items.append((ws, emit, (E.sem, 1)))
        self._commit(tok, reads, writes)
        return tok

    def dma(self, queue, out_ap, in_ap, reads, writes, **kw):
        E = self.eng[queue]
        dst = writes[0]
        if dst.sem is None:
            if self.free_sems:
                dst.sem, dst.sem_count = self.free_sems.pop()
            else:
                dst.sem = self.new_sem('d%d' % len(self.sems))
            self.all_dma_bufs.append(dst)
        ws = self._waits(E, self._deps(reads, writes))
        dst.sem_count += 16
        tok = (dst.sem, dst.sem_count, dict(E.known))
        E.items.append((ws, (lambda e: e.dma_start(out=out_ap, in_=in_ap, **kw)), (dst.sem, 16)))
        self._commit(tok, reads, writes)
        return tok

    def dma_custom(self, queue, emit, reads, writes, inc=16):
        E = self.eng[queue]
        dst = writes[0]
        if dst.sem is None:
            if self.free_sems:
                dst.sem, dst.sem_count = self.free_sems.pop()
            else:
                dst.sem = self.new_sem('d%d' % len(self.sems))
            self.all_dma_bufs.append(dst)
        ws = self._waits(E, self._deps(reads, writes))
        dst.sem_count += inc
        tok = (dst.sem, dst.sem_count, dict(E.known))
        E.items.append((ws, emit, (dst.sem, inc)))
        self._commit(tok, reads, writes)
        return tok

    def barrier(self):
        deps = []
        for n in self.ENGS:
            E = self.eng[n]
            if E.count:
                deps.append((E.sem, E.count, {}))
        seen = set()
        for b in self.all_dma_bufs:
            if b.sem is not None and b.sem not in seen:
                seen.add(b.sem)
                deps.append((b.sem, b.sem_count, {}))
        for (s, c) in self.free_sems:
            if s not in seen:
                seen.add(s)
                deps.append((s, c, {}))
        for n in self.ENGS:
            E = self.eng[n]
            ws = []
            for (s, v, _) in deps:
                if E.known.get(s, 0) < v and not (n == 'tensor' and s == E.sem):
                    ws.append((s, v))
                    E.known[s] = v
            if ws:
                E.items.append((ws, None, None))

    def rotate(self, limit=20000):
        self.barrier()
        for n in self.ENGS:
            E = self.eng[n]
            if E.count > limit:
                E.sem = self.new_sem('e2_%s_%d' % (n, len(self.sems)))
                E.count = 0

    def finish(self):
        self.barrier()
        sems = self.sems
        with self.nc.Block() as block:
            for n in self.ENGS:
                items = self.eng[n].items

                def body(e, items=items):
                    for ws, emit, inc in items:
                        for s, v in ws:
                            e.wait_ge(sems[s], v)
                        if emit is not None:
                            ins = emit(e)
                            ins.then_inc(sems[inc[0]], inc[1])
                getattr(block, n)(body)
        self.stack.close()
        return sum(len(self.eng[n].items) for n in self.ENGS)


class Prog:
    def __init__(self, L, NQ, C, stage):
        self.L, self.NQ, self.C, self.stage = L, NQ, C, stage
        self.NK = L + C
        self.NT = self.NK // 128
        nc = bass.Bass("TRN2", target_bir_lowering=False)
        self.nc = nc
        self.S = Sched(nc, arena_bytes=200 * 1024)
        self.inputs = {}
        self.outputs = {}
        S = self.S
        self.PS = [S.psum("ps%d" % i, [128, 512], F32) for i in range(8)]
        self.tr_ctr = 0

    def inp(self, name, shape, dtype=F32):
        t = self.nc.dram_tensor(name, list(shape), dtype, kind="ExternalInput")
        self.inputs[name] = (tuple(shape), dtype)
        return t.ap()

    def outp(self, name, shape, dtype=F32):
        t = self.nc.dram_tensor(name, list(shape), dtype, kind="ExternalOutput")
        self.outputs[name] = (tuple(shape), dtype)
        return t.ap()

    def scratch(self, name, shape, dtype):
        return self.nc.dram_tensor(name, list(shape), dtype).ap()

    def mm(self, ps, out_ap, lhs, rhs, reads, start=True, stop=True):
        def emit(e):
            n = len(lhs)
            for i in range(n):
                ins = e.matmul(out_ap, lhsT=lhs[i], rhs=rhs[i], start=(start and i == 0), stop=(stop and i == n - 1))
            return ins
        return self.S.op('tensor', reads, [ps], emit)

    def act(self, out_b, out_ap, in_b, in_ap, func, reads=(), **kw):
        return self.S.op('scalar', [in_b] + list(reads), [out_b],
                         lambda e: e.activation(out=out_ap, in_=in_ap, func=func, **kw))

    def tt(self, eng, out_b, out_ap, a_b, a_ap, b_b, b_ap, op):
        return self.S.op(eng, [a_b, b_b], [out_b],
                         lambda e: e.tensor_tensor(out=out_ap, in0=a_ap, in1=b_ap, op=op))

    def stt(self, out_b, out_ap, a_b, a_ap, scalar, b_b, b_ap, op0, op1, reads=(), writes=(), **kw):
        return self.S.op('vector', [a_b, b_b] + list(reads), [out_b] + list(writes),
                         lambda e: e.scalar_tensor_tensor(out=out_ap, in0=a_ap, scalar=scalar, in1=b_ap, op0=op0, op1=op1, **kw))

    def ts(self, eng, out_b, out_ap, a_b, a_ap, s1, s2, op0, op1=None, reads=()):
        if op1 is None:
            return self.S.op(eng, [a_b] + list(reads), [out_b],
                             lambda e: e.tensor_scalar(out=out_ap, in0=a_ap, scalar1=s1, scalar2=None, op0=op0))
        return self.S.op(eng, [a_b] + list(reads), [out_b],
                         lambda e: e.tensor_scalar(out=out_ap, in0=a_ap, scalar1=s1, scalar2=s2, op0=op0, op1=op1))

    def cp(self, eng, out_b, out_ap, in_b, in_ap):
        if eng == 'scalar':
            return self.S.op(eng, [in_b], [out_b], lambda e: e.copy(out=out_ap, in_=in_ap))
        return self.S.op(eng, [in_b], [out_b], lambda e: e.tensor_copy(out=out_ap, in_=in_ap))

    def memset(self, eng, b, ap, val):
        return self.S.op(eng, [], [b], lambda e: e.memset(ap, val))

    def load(self, b, out_ap, in_ap, src=None, q='sync', **kw):
        return self.S.dma(q, out_ap, in_ap, [src] if src is not None else [], [b], **kw)

    def store(self, dst, out_ap, b, in_ap, q='sync', **kw):
        return self.S.dma(q, out_ap, in_ap, [b], [dst], **kw)

    def setup_consts(self):
        S = self.S
        self.ident = S.alloc("ident", [128], BF16)
        self.rmat = S.alloc("rmat", [128], BF16)
        self.onesb = S.alloc("onesb", [128], BF16)
        self.epsc = S.alloc("epsc", [1], F32)
        c_ident = self.inp("c_ident", [128, 128])
        c_rmat = self.inp("c_rmat", [128, 128])
        self.load(self.ident, self.ident.ap[:], c_ident, q='gpsimd')
        self.load(self.rmat, self.rmat.ap[:], c_rmat, q='gpsimd')
        self.memset('vector', self.onesb, self.onesb.ap[:], 1.0)
        self.memset('vector', self.epsc, self.epsc.ap[:], EPS)

    def mod_prologue(self, layers):
        S = self.S
        m0 = S.mark()
        cc = self.inp("cc", [128, 8, 2])
        mod_w = self.inp("mod_w", [2, D, 6 * D])
        mod_b = self.inp("mod_b", [2, 6 * D])
        n1g = self.inp("norm1_g", [2, D])
        n2g = self.inp("norm2_g", [2, D])
        self.MODB = self.scratch("MODB", [2, 2, 6, D], F32)
        self.modb_buf = S.dram_buf("MODB")
        ccs = S.alloc("ccs", [8, 2], F32)
        sct = S.alloc("sct", [8, 2], F32)
        self.load(ccs, ccs.ap[:], cc)
        self.act(sct, sct.ap[:], ccs, ccs.ap[:], AF.Silu)
        mw = [S.alloc("mw%d" % i, [8, 512], F32) for i in range(2)]
        row = S.alloc("modrow", [6 * D], F32)
        brow = S.alloc("modbrow", [6 * D], F32)
        g1r = S.alloc("g1r", [D], F32)
        g2r = S.alloc("g2r", [D], F32)
        orow = S.alloc("orow", [6, D], F32)
        ps = self.PS
        k = 0
        for i in layers:
            self.load(brow, brow.ap[0:2, :], mod_b[i:i + 1, :].partition_broadcast(2))
            self.load(g1r, g1r.ap[0:2, :], n1g[i:i + 1, :].partition_broadcast(2))
            self.load(g2r, g2r.ap[0:2, :], n2g[i:i + 1, :].partition_broadcast(2))
            for n in range(12):
                w = mw[k % 2]
                self.load(w, w.ap[:], mod_w[i, :, n * 512:(n + 1) * 512].rearrange("(c p) n -> p c n", p=128))
                pb = ps[k % 2]
                self.mm(pb, pb.ap[0:2, :], [sct.ap[:, c, :] for c in range(8)], [w.ap[:, c, :] for c in range(8)], [sct, w])
                self.tt('vector', row, row.ap[0:2, n * 512:(n + 1) * 512], pb, pb.ap[0:2, :], brow, brow.ap[0:2, n * 512:(n + 1) * 512], ALU.add)
                k += 1
            r = lambda j: row.ap[0:2, j * D:(j + 1) * D]
            self.stt(orow, orow.ap[0:2, 0, :], row, r(1), 1.0, g1r, g1r.ap[0:2, :], ALU.add, ALU.mult)
            self.cp('vector', orow, orow.ap[0:2, 1, :], row, r(0))
            self.cp('vector', orow, orow.ap[0:2, 2, :], row, r(2))
            self.stt(orow, orow.ap[0:2, 3, :], row, r(4), 1.0, g2r, g2r.ap[0:2, :], ALU.add, ALU.mult)
            self.cp('vector', orow, orow.ap[0:2, 4, :], row, r(3))
            self.cp('vector', orow, orow.ap[0:2, 5, :], row, r(5))
            self.store(self.modb_buf, self.MODB[i], orow, orow.ap[0:2, :, :])
        S.barrier()
        S.release(m0)

    def load_mod(self, name, layer, which, rowi):
        b = self.S.alloc(name, [D], F32)
        self.load(b, b.ap[:], self.MODB[layer, which, rowi:rowi + 1, :].partition_broadcast(128), src=self.modb_buf)
        return b

    def alloc_norm_tmp(self, tag):
        S = self.S
        t = {}
        t['junk'] = S.alloc(tag + "junk", [D], BF16)
        t['ss'] = [S.alloc(tag + "ss%d" % i, [8], F32, multi=True) for i in range(2)]
        t['tmp'] = [S.alloc(tag + "tmp%d" % i, [D], F32) for i in range(2)]
        t['ab'] = [S.alloc(tag + "ab%d" % i, [D], BF16) for i in range(2)]
        t['af'] = [S.alloc(tag + "af%d" % i, [D], F32) for i in range(2)]
        t['k'] = 0
        return t

    def norm_mod_T(self, t, xt, A, B, aT, want_f32=False, tr_banks=(0, 1)):
        n = len(xt)
        ssb = t['ss'][t['k'] % 2]
        afs = []
        for i, (xb, xap) in enumerate(xt):
            self.S.op('scalar', [xb], [t['junk'], ssb],
                      lambda e, xap=xap, i=i: e.activation(out=t['junk'].ap[:], in_=xap, func=AF.Square, accum_out=ssb.ap[:, i:i + 1]))
        self.act(ssb, ssb.ap[:, 4:4 + n], ssb, ssb.ap[:, 0:n], AF.Sqrt, reads=[self.epsc], scale=1.0 / D, bias=self.epsc.ap[:])
        self.S.op('vector', [ssb], [ssb], lambda e: e.reciprocal(out=ssb.ap[:, 4:4 + n], in_=ssb.ap[:, 4:4 + n]))
        for i, (xb, xap) in enumerate(xt):
            k = t['k']
            t['k'] += 1
            tmp = t['tmp'][k % 2]
            ab = t['ab'][k % 2]
            self.stt(tmp, tmp.ap[:], xb, xap, ssb.ap[:, 4 + i:5 + i], A, A.ap[:], ALU.mult, ALU.mult, reads=[ssb])
            if want_f32:
                af = t['af'][k % 2]
                self.tt('gpsimd', af, af.ap[:], tmp, tmp.ap[:], B, B.ap[:], ALU.add)
                self.cp('scalar', ab, ab.ap[:], af, af.ap[:])
                afs.append(af)
            else:
                self.tt('gpsimd', ab, ab.ap[:], tmp, tmp.ap[:], B, B.ap[:], ALU.add)
            pb = self.PS[tr_banks[self.tr_ctr % 2]]
            self.tr_ctr += 1
            pv = pb.ap[:].bitcast(BF16).rearrange("p (c n) -> p c n", c=8)

            def tr(e, ab=ab, pv=pv):
                for c in range(8):
                    ins = e.transpose(out=pv[:, c, :], in_=ab.ap[:, c * 128:(c + 1) * 128], identity=self.ident.ap[:])
                return ins
            self.S.op('tensor', [ab, self.ident], [pb], tr)
            ob, oap = aT[i]
            self.cp('scalar', ob, oap, pb, pv)
            if want_f32:
                yield af

    def run_norm(self, *a, **kw):
        return list(self.norm_mod_T(*a, **kw))

    def rope_tile(self, rows, G, zp, kz, rp, t1, t2, cosb, cos_ap, sinb, sin_ap, ob, oap):
        self.cp('scalar', kz, kz.ap[0:rows, 0:G], zp, zp.ap[0:rows, 0:G])
        self.mm(rp, rp.ap[0:rows, 0:G], [self.rmat.ap[0:rows, 0:rows]], [kz.ap[0:rows, 0:G]], [self.rmat, kz])
        self.tt('vector', t1, t1.ap[0:rows, 0:G], kz, kz.ap[0:rows, 0:G], cosb, cos_ap, ALU.mult)
        self.tt('vector', t2, t2.ap[0:rows, 0:G], rp, rp.ap[0:rows, 0:G], sinb, sin_ap, ALU.mult)
        self.tt('gpsimd', ob, oap, t1, t1.ap[0:rows, 0:G], t2, t2.ap[0:rows, 0:G], ALU.add)

    def l0_kv_build(self):
        S, L, C = self.S, self.L, self.C
        m0 = S.mark()
        xb = self.inp("xb", [L, D])
        ctxb = self.inp("ctxb", [C, D])
        ropeK = self.inp("ropeK", [2, 128, L])
        self.w_in0 = self.inp("even_w_in", [D, 2560])
        self.KT0 = self.scratch("KT0", [6, 128, self.NK], BF16)
        self.V0 = self.scratch("V0", [6, 128, self.NT, 128], BF16)
        self.kt0_buf = S.dram_buf("KT0")
        self.v0_buf = S.dram_buf("V0")
        Wk = S.alloc("Wk", [8, 768], BF16)
        Wv = S.alloc("Wv", [8, 768], BF16)
        self.load(Wk, Wk.ap[:], self.w_in0[:, 1024:1792].rearrange("(c p) n -> p c n", p=128), q='gpsimd')
        self.load(Wv, Wv.ap[:], self.w_in0[:, 1792:2560].rearrange("(c p) n -> p c n", p=128), q='gpsimd')
        A = [self.load_mod("kvA%d" % w, 0, w, 0) for w in range(2)]
        B = [self.load_mod("kvB%d" % w, 0, w, 1) for w in range(2)]
        t = self.alloc_norm_tmp("kv")
        xg = [S.alloc("kvx%d" % i, [4, D], F32) for i in range(2)]
        aT = [S.alloc("kvaT%d" % i, [8, 512], BF16) for i in range(2)]
        cs = [S.alloc("kvcs%d" % i, [2, 512], F32) for i in range(2)]
        kz = [S.alloc("kvkz%d" % i, [512], BF16) for i in range(2)]
        t1 = [S.alloc("kvt1%d" % i, [512], F32) for i in range(2)]
        t2 = [S.alloc("kvt2%d" % i, [512], F32) for i in range(2)]
        kst = [S.alloc("kvkst%d" % i, [512], BF16) for i in range(4)]
        vst = [S.alloc("kvvst%d" % i, [6, 4, 128], BF16) for i in range(2)]
        PS = self.PS
        groups = []
        for g in range(L // 512):
            groups.append((0, g * 512, 512))
        for g in range((C + 511) // 512):
            groups.append((1, g * 512, min(512, C - g * 512)))

        def issue_load(gi):
            which, t0, G = groups[gi]
            nt = G // 128
            src = xb if which == 0 else ctxb
            xs = xg[gi % 2]
            self.load(xs, xs.ap[:, 0:nt, :], src[t0:t0 + G, :].rearrange("(t p) d -> p t d", p=128))
            if which == 0:
                c_ = cs[gi % 2]
                self.load(c_, c_.ap[:, :, 0:G], ropeK[:, :, t0:t0 + G].rearrange("a p n -> p a n"))
        issue_load(0)
        kk = 0
        for gi, (which, t0, G) in enumerate(groups):
            if gi + 1 < len(groups):
                issue_load(gi + 1)
            nt = G // 128
            xs = xg[gi % 2]
            aTs = aT[gi % 2]
            c_ = cs[gi % 2]
            self.run_norm(t, [(xs, xs.ap[:, i, :]) for i in range(nt)], A[which], B[which],
                          [(aTs, aTs.ap[:, :, i * 128:(i + 1) * 128]) for i in range(nt)])
            key0 = t0 if which == 0 else L + t0
            for h in range(6):
                zp = PS[2 + kk % 2]
                rp = PS[4 + kk % 2]
                ks = kst[kk % 4]
                self.mm(zp, zp.ap[:, 0:G], [Wk.ap[:, c, h * 128:(h + 1) * 128] for c in range(8)],
                        [aTs.ap[:, c, 0:G] for c in range(8)], [Wk, aTs])
                if which == 0:
                    self.rope_tile(128, G, zp, kz[kk % 2], rp, t1[kk % 2], t2[kk % 2],
                                   c_, c_.ap[:, 0, 0:G], c_, c_.ap[:, 1, 0:G], ks, ks.ap[:, 0:G])
                else:
                    self.cp('scalar', ks, ks.ap[:, 0:G], zp, zp.ap[:, 0:G])
                self.store(self.kt0_buf, self.KT0[h, :, key0:key0 + G], ks, ks.ap[:, 0:G])
                kk += 1
            vs = vst[gi % 2]
            for i in range(nt):
                va, vb = PS[6], PS[7]
                lhs = [aTs.ap[:, c, i * 128:(i + 1) * 128] for c in range(8)]
                self.mm(va, va.ap[:, 0:512], lhs, [Wv.ap[:, c, 0:512] for c in range(8)], [Wv, aTs])
                self.mm(vb, vb.ap[:, 0:256], lhs, [Wv.ap[:, c, 512:768] for c in range(8)], [Wv, aTs])
                self.cp('vector', vs, vs.ap[:, 0:4, i, :], va, va.ap[:, 0:512].rearrange("p (h d) -> p h d", h=4))
                self.cp('vector', vs, vs.ap[:, 4:6, i, :], vb, vb.ap[:, 0:256].rearrange("p (h d) -> p h d", h=2))
            tile0 = key0 // 128
            self.store(self.v0_buf, self.V0[:, :, tile0:tile0 + nt, :].rearrange("h p t d -> p h t d"), vs, vs.ap[:, :, 0:nt, :])
        S.barrier()
        S.release(m0)

    def l0_q_build(self, tag, xq, nq, xh, hmask, which, ropeQ, QTs, qt_buf, pext):
        S = self.S
        m0 = S.mark()
        qst = [S.alloc("qst%d" % i, [512], BF16) for i in range(4)]
        Wq = S.alloc("Wq", [8, 768], BF16)
        Wp = S.alloc("Wp", [8, 256], BF16)
        self.load(Wq, Wq.ap[:], self.w_in0[:, 256:1024].rearrange("(c p) n -> p c n", p=128), q='gpsimd')
        self.load(Wp, Wp.ap[:], self.w_in0[:, 0:256].rearrange("(c p) n -> p c n", p=128), q='gpsimd')
        A = self.load_mod("qA", 0, which, 0)
        B = self.load_mod("qB", 0, which, 1)
        t = self.alloc_norm_tmp("q")
        xg = [S.alloc("qx%d" % i, [4, D], F32) for i in range(2)]
        aT = [S.alloc("qaT%d" % i, [8, 512], BF16) for i in range(2)]
        cs = [S.alloc("qcs%d" % i, [2, 512], F32) for i in range(2)]
        kz = [S.alloc("qkz%d" % i, [512], BF16) for i in range(2)]
        t1 = [S.alloc("qt1%d" % i, [512], F32) for i in range(2)]
        t2 = [S.alloc("qt2%d" % i, [512], F32) for i in range(2)]
        hm = S.alloc("qhm", [16], F32)
        self.load(hm, hm.ap[:], hmask)
        PS = self.PS
        ngr = (nq + 511) // 512
        groups = [(g * 512, min(512, nq - g * 512)) for g in range(ngr)] + [(-1, 128)]

        def issue_load(gi):
            t0, G = groups[gi]
            xs = xg[gi % 2]
            if t0 < 0:
                self.load(xs, xs.ap[:, 0:1, :], xh.rearrange("(t p) d -> p t d", p=128))
                return
            self.load(xs, xs.ap[:, 0:G // 128, :], xq[t0:t0 + G, :].rearrange("(t p) d -> p t d", p=128))
            if ropeQ is not None:
                c_ = cs[gi % 2]
                self.load(c_, c_.ap[:, :, 0:G], ropeQ[:, :, t0:t0 + G].rearrange("a p n -> p a n"))
        issue_load(0)
        kk = 0
        for gi, (t0, G) in enumerate(groups):
            if gi + 1 < len(groups):
                issue_load(gi + 1)
            nt = G // 128
            xs = xg[gi % 2]
            aTs = aT[gi % 2]
            c_ = cs[gi % 2]
            self.run_norm(t, [(xs, xs.ap[:, i, :]) for i in range(nt)], A, B,
                          [(aTs, aTs.ap[:, :, i * 128:(i + 1) * 128]) for i in range(nt)])
            if t0 < 0:
                for j in range(2):
                    zp = PS[2 + j]
                    self.mm(zp, zp.ap[:, 0:16], [Wp.ap[:, c, j * 128:(j + 1) * 128] for c in range(8)],
                            [aTs.ap[:, c, 0:16] for c in range(8)], [Wp, aTs])
                    self.tt('vector', pext, pext.ap[:, j, 0:8], zp, zp.ap[:, 0:8], hm, hm.ap[:, 0:8], ALU.mult)
                    self.tt('vector', pext, pext.ap[:, j, 8 + nq:16 + nq], zp, zp.ap[:, 8:16], hm, hm.ap[:, 8:16], ALU.mult)
                continue
            for j in range(2):
                zp = PS[2 + kk % 2]
                self.mm(zp, zp.ap[:, 0:G], [Wp.ap[:, c, j * 128:(j + 1) * 128] for c in range(8)],
                        [aTs.ap[:, c, 0:G] for c in range(8)], [Wp, aTs])
                self.cp('scalar', pext, pext.ap[:, j, 8 + t0:8 + t0 + G], zp, zp.ap[:, 0:G])
                kk += 1
            for h in range(6):
                zp = PS[2 + kk % 2]
                rp = PS[4 + kk % 2]
                self.mm(zp, zp.ap[:, 0:G], [Wq.ap[:, c, h * 128:(h + 1) * 128] for c in range(8)],
                        [aTs.ap[:, c, 0:G] for c in range(8)], [Wq, aTs])
                qb = qst[kk % 4]
                if ropeQ is not None:
                    self.rope_tile(128, G, zp, kz[kk % 2], rp, t1[kk % 2], t2[kk % 2],
                                   c_, c_.ap[:, 0, 0:G], c_, c_.ap[:, 1, 0:G], qb, qb.ap[:, 0:G])
                else:
                    self.cp('scalar', qb, qb.ap[:, 0:G], zp, zp.ap[:, 0:G])
                self.store(qt_buf, QTs[h, :, t0:t0 + G], qb, qb.ap[:, 0:G])
                kk += 1
        S.barrier()
        S.release(m0)

    def l0_pool(self, nq, pext, icnt_dram, OT, ot_buf):
        S = self.S
        m0 = S.mark()
        n = nq
        sA = S.alloc("plA", [2, n + 16], F32)
        sB = S.alloc("plB", [2, n + 16], F32)
        ic = S.alloc("plic", [2, n], F32)
        pd = S.alloc("plpd", [2, n], BF16)
        pw = S.alloc("plw", [2, 128], BF16)
        psc = S.alloc("plsc", [2], F32)
        pool_w = self.inp_once("pool_w", [4, 64, 64])
        pool_scale = self.inp_once("pool_scale", [256])
        self.load(ic, ic.ap[:], icnt_dram)
        self.memset('vector', pw, pw.ap[:], 0.0)
        for g in range(4):
            j, hf = g // 2, g % 2
            self.load(pw, pw.ap[hf * 64:(hf + 1) * 64, j, hf * 64:(hf + 1) * 64], pool_w[g], q='gpsimd')
        self.load(psc, psc.ap[:], pool_scale.rearrange("(j p) -> p j", p=128), allow_slow_non_contiguous=True)
        eng = 'gpsimd'
        for g, w in enumerate(POOL_WINDOWS):
            j, hf = g // 2, g % 2
            r = slice(hf * 64, (hf + 1) * 64)
            u = pext.ap[r, j, :]
            a = sA.ap[r, j, :]
            b = sB.ap[r, j, :]
            N = n + 16
            self.tt(eng, sA, a[:, 1:N], pext, u[:, 0:N - 1], pext, u[:, 1:N], ALU.add)
            fin, finb = a, sA
            if w >= 4:
                self.tt(eng, sB, b[:, 2:N - 1], sA, a[:, 1:N - 2], sA, a[:, 3:N], ALU.add)
                fin, finb = b, sB
            if w >= 8:
                self.tt(eng, sA, a[:, 4:N - 3], sB, b[:, 2:N - 5], sB, b[:, 6:N - 1], ALU.add)
                fin, finb = a, sA
            if w >= 16:
                self.tt(eng, sB, b[:, 8:N - 7], sA, a[:, 4:N - 11], sA, a[:, 12:N - 3], ALU.add)
                fin, finb = b, sB
            other = sB if finb is sA else sA
            oth = other.ap[r, j, :]
            self.tt('vector', other, oth[:, 8:8 + n], finb, fin[:, 8:8 + n], ic, ic.ap[r, j, :], ALU.mult)
            self.tt('vector', pd, pd.ap[r, j, :], other, oth[:, 8:8 + n], pext, u[:, 8:8 + n], ALU.subtract)
        stg = [S.alloc("plst%d" % i, [512], BF16) for i in range(2)]
        kk = 0
        for j in range(2):
            for t0 in range(0, n, 512):
                G = min(512, n - t0)
                zp = self.PS[kk % 2]
                st = stg[kk % 2]
                self.mm(zp, zp.ap[:, 0:G], [pw.ap[:, j, :]], [pd.ap[:, j, t0:t0 + G]], [pw, pd])
                self.act(st, st.ap[:, 0:G], zp, zp.ap[:, 0:G], AF.Copy, reads=[psc], scale=psc.ap[:, j:j + 1])
                self.store(ot_buf, OT[j, :, t0:t0 + G], st, st.ap[:, 0:G])
                kk += 1
        S.barrier()
        S.release(m0)

    def inp_once(self, name, shape, dtype=F32):
        if not hasattr(self, '_inp_cache'):
            self._inp_cache = {}
        if name not in self._inp_cache:
            self._inp_cache[name] = self.inp(name, shape, dtype)
        return self._inp_cache[name]

    def attention(self, cfg):
        S = self.S
        nh, nq, key0, nkeys, maps = cfg['nh'], cfg['nq'], cfg['key0'], cfg['nkeys'], cfg['maps']
        ntl = nkeys // 128
        nt0 = (ntl + 1) // 2
        halves = [(0, nt0), (nt0, ntl)] if ntl > 1 else [(0, 1)]
        m0 = S.mark()
        Kb = [S.alloc("atK%d" % i, [(b - a) * 128], BF16, multi=True) for i, (a, b) in enumerate(halves)]
        Vb = [S.alloc("atV%d" % i, [(b - a), 128], BF16, multi=True) for i, (a, b) in enumerate(halves)]
        lag = 2 if maps == 1 else 1
        NPT = 4 if maps == 2 else 6
        PT = [[S.alloc("atP%d_%d" % (m, i), [512], BF16) for i in range(NPT)] for m in range(maps)]
        SA = [[S.alloc("atSA%d_%d" % (m, i), [512], BF16) for i in range(2)] for m in range(maps)]
        SB = [[S.alloc("atSB%d_%d" % (m, i), [512], BF16) for i in range(2)] for m in range(maps)]
        SC = [[S.alloc("atSC%d_%d" % (m, i), [512], BF16) for i in range(2)] for m in range(maps)]
        PS = self.PS
        if maps == 2:
            Sb = [[PS[0], PS[1]], [PS[2], PS[3]]]
            Ob = [[PS[4]], [PS[5]]]
            Lb = [[PS[6]], [PS[7]]]
        else:
            Sb = [[PS[0], PS[1], PS[6]]]
            Ob = [[PS[2], PS[3]]]
            Lb = [[PS[4], PS[5]]]
        cfg['free_banks'] = [PS[6], PS[7]] if maps == 1 else []
        scale = cfg['scale']
        pctr = 0
        cctr = 0
        for h in range(nh):
            KT = cfg['KT'](h)
            V = cfg['V'](h)
            for i, (a, b) in enumerate(halves):
                self.load(Kb[i], Kb[i].ap[:, :], KT[:, key0 + a * 128:key0 + b * 128], src=cfg['kbuf'])
                self.load(Vb[i], Vb[i].ap[:, :, :], V[:, key0 // 128 + a:key0 // 128 + b, :], src=cfg['vbuf'])
            qh = cfg['qprov'](h)
            nchunk = (nq + 511) // 512
            for qc in range(nchunk):
                G = min(512, nq - qc * 512)
                qinfo = qh(qc)
                O = [Ob[m][cctr % len(Ob[m])] for m in range(maps)]
                Lq = [Lb[m][cctr % len(Lb[m])] for m in range(maps)]
                cctr += 1
                pq = []
                hist = [[] for _ in range(maps)]
                pendL = [[] for _ in range(maps)]
                for kt in range(ntl + lag):
                    cur = None
                    if kt < ntl:
                        hi = 0 if kt < halves[0][1] else 1
                        kl = kt - halves[hi][0]
                        cur = []
                        for m in range(maps):
                            sb = Sb[m][kt % len(Sb[m])]
                            pt = PT[m][pctr % NPT]
                            parts = qinfo[m]

                            def emit(e, parts=parts, sb=sb, hi=hi, kl=kl, G=G):
                                n = len(parts)
                                for i, (kind, qb, qap, rows) in enumerate(parts):
                                    if kind == 'main':
                                        lhs = Kb[hi].ap[rows[0]:rows[1], kl * 128:(kl + 1) * 128]
                                    else:
                                        lhs = cfg['KR'].ap[rows[0]:rows[1], key0 + (halves[hi][0] + kl) * 128:key0 + (halves[hi][0] + kl + 1) * 128]
                                    ins = e.matmul(sb.ap[:, 0:G], lhsT=lhs, rhs=qap, start=(i == 0), stop=(i == n - 1))
                                return ins
                            rd = [Kb[hi]] + [p[1] for p in parts] + ([cfg['KR']] if any(p[0] == 'rope' for p in parts) else [])
                            S.op('tensor', rd, [sb], emit)
                            self.act(pt, pt.ap[:, 0:G], sb, sb.ap[:, 0:G], AF.Exp, scale=scale)
                            cur.append((pt, hi, kl))
                        pctr += 1
                        pq.append(cur)
                    if kt >= lag:
                        ktp = kt - lag
                        pend = pq.pop(0)
                        for m in range(maps):
                            pt, hi, kl = pend[m]
                            self.mm(O[m], O[m].ap[:, 0:G], [Vb[hi].ap[:, kl, :]], [pt.ap[:, 0:G]], [Vb[hi], pt],
                                    start=(ktp == 0), stop=(ktp == ntl - 1))
                            gpos = ktp % 4
                            gidx = ktp // 4
                            gsize = min(4, ntl - gidx * 4)
                            hist[m].append(pt)
                            if gpos == 1:
                                a_ = SA[m][gidx % 2]
                                self.tt('vector', a_, a_.ap[:, 0:G], hist[m][0], hist[m][0].ap[:, 0:G], hist[m][1], hist[m][1].ap[:, 0:G], ALU.add)
                            if gpos == gsize - 1:
                                tl = hist[m]
                                hist[m] = []
                                if gsize == 1:
                                    src = tl[0]
                                elif gsize == 2:
                                    src = SA[m][gidx % 2]
                                elif gsize == 3:
                                    src = SC[m][gidx % 2]
                                    a_ = SA[m][gidx % 2]
                                    self.tt('vector', src, src.ap[:, 0:G], a_, a_.ap[:, 0:G], tl[2], tl[2].ap[:, 0:G], ALU.add)
                                else:
                                    a_ = SA[m][gidx % 2]
                                    b_ = SB[m][gidx % 2]
                                    src = SC[m][gidx % 2]
                                    self.tt('vector', b_, b_.ap[:, 0:G], tl[2], tl[2].ap[:, 0:G], tl[3], tl[3].ap[:, 0:G], ALU.add)
                                    self.tt('vector', src, src.ap[:, 0:G], a_, a_.ap[:, 0:G], b_, b_.ap[:, 0:G], ALU.add)
                                pendL[m].append((src, gidx, kt))
                            ngrp = (ntl + 3) // 4
                            while pendL[m] and (kt - pendL[m][0][2] >= 3 or ktp == ntl - 1):
                                src_, gi_, _ = pendL[m].pop(0)
                                self.mm(Lq[m], Lq[m].ap[:, 0:G], [self.onesb.ap[:]], [src_.ap[:, 0:G]], [self.onesb, src_],
                                        start=(gi_ == 0), stop=(gi_ == ngrp - 1))
                cfg['epilogue'](h, qc, G, O, Lq)
        S.barrier()
        S.release(m0)

    def l0_attention(self, nq, key0, nkeys, QTs, qt_buf, OT, ot_buf, lam_init):
        S = self.S
        m0 = S.mark()
        lamp = self.inp_once("lam", [4, 64])
        lt = S.alloc("lamt", [256], F32)
        qh_b = [S.alloc("l0q%d" % i, [nq], BF16) for i in range(2)]
        ls = S.alloc("lams", [8], F32)
        lj = S.alloc("lamj", [64], F32)
        self.load(lt, lt.ap[:], lamp.rearrange("a n -> (a n)").rearrange("(o n) -> o n", o=1).partition_broadcast(128))
        self.stt(lj, lj.ap[:], lt, lt.ap[:, 0:64], 1.0, lt, lt.ap[:, 64:128], ALU.mult, ALU.mult, writes=[ls], accum_out=ls.ap[:, 0:1])
        self.stt(lj, lj.ap[:], lt, lt.ap[:, 128:192], 1.0, lt, lt.ap[:, 192:256], ALU.mult, ALU.mult, writes=[ls], accum_out=ls.ap[:, 1:2])
        self.act(ls, ls.ap[:, 2:4], ls, ls.ap[:, 0:2], AF.Exp)
        self.stt(ls, ls.ap[:, 4:5], ls, ls.ap[:, 3:4], -lam_init, ls, ls.ap[:, 2:3], ALU.add, ALU.subtract)
        nlam = ls.ap[:, 4:5]
        r1 = S.alloc("epr1", [512], F32)
        r2 = S.alloc("epr2", [512], F32)
        t1 = S.alloc("ept1", [512], F32)
        t2 = S.alloc("ept2", [512], F32)
        at = S.alloc("epat", [512], F32)
        sq = S.alloc("epsq", [512], BF16)
        rs = S.alloc("eprs", [512], F32)
        hd = [S.alloc("ephd%d" % i, [512], BF16) for i in range(2)]
        ectr = [0]

        def epilogue(h, qc, G, O, Lq):
            self.S.op('vector', [Lq[0]], [r1], lambda e: e.reciprocal(out=r1.ap[:, 0:G], in_=Lq[0].ap[:, 0:G]))
            self.tt('vector', t1, t1.ap[:, 0:G], O[0], O[0].ap[:, 0:G], r1, r1.ap[:, 0:G], ALU.mult)
            self.S.op('vector', [Lq[1]], [r2], lambda e: e.reciprocal(out=r2.ap[:, 0:G], in_=Lq[1].ap[:, 0:G]))
            self.tt('vector', t2, t2.ap[:, 0:G], O[1], O[1].ap[:, 0:G], r2, r2.ap[:, 0:G], ALU.mult)
            self.stt(at, at.ap[:, 0:G], t2, t2.ap[:, 0:G], nlam, t1, t1.ap[:, 0:G], ALU.mult, ALU.add, reads=[ls])
            self.tt('gpsimd', sq, sq.ap[:, 0:G], at, at.ap[:, 0:G], at, at.ap[:, 0:G], ALU.mult)
            zp = Lq[0]
            self.mm(zp, zp.ap[:, 0:G], [self.onesb.ap[:]], [sq.ap[:, 0:G]], [self.onesb, sq])
            self.act(rs, rs.ap[:, 0:G], zp, zp.ap[:, 0:G], AF.Sqrt, reads=[self.epsc], scale=1.0 / 128, bias=self.epsc.ap[:])
            self.S.op('vector', [rs], [rs], lambda e: e.reciprocal(out=rs.ap[:, 0:G], in_=rs.ap[:, 0:G]))
            o = hd[ectr[0] % 2]
            ectr[0] += 1
            self.stt(o, o.ap[:, 0:G], at, at.ap[:, 0:G], 1.0 - lam_init, rs, rs.ap[:, 0:G], ALU.mult, ALU.mult)
            self.store(ot_buf, OT[2 + h, :, qc * 512:qc * 512 + G], o, o.ap[:, 0:G])

        def qprov(h):
            qb = qh_b[h % 2]
            self.load(qb, qb.ap[:], QTs[h], src=qt_buf)

            def f(qc):
                G = min(512, nq - qc * 512)
                q0 = qc * 512
                return [[('main', qb, qb.ap[0:64, q0:q0 + G], (0, 64))], [('main', qb, qb.ap[64:128, q0:q0 + G], (64, 128))]]
            return f
        cfg = dict(nh=6, nq=nq, key0=key0, nkeys=nkeys, maps=2, scale=64 ** -0.5,
                   KT=lambda h: self.KT0[h], V=lambda h: self.V0[h], kbuf=self.kt0_buf, vbuf=self.v0_buf,
                   qprov=qprov, epilogue=epilogue)
        self.attention(cfg)
        S.barrier()
        S.release(m0)

    def x_phase(self, tag, layer, which, nq, OT, ot_buf, res_src, res_buf, w_out, H1, h1_buf, A2T, a2t_buf, router=None):
        S = self.S
        m0 = S.mark()
        Wo = S.alloc("xWo", [8, D], BF16)
        self.load(Wo, Wo.ap[:], w_out.rearrange("(c p) n -> p c n", p=128), q='gpsimd')
        G1 = self.load_mod("xG1", layer, which, 2)
        A2 = self.load_mod("xA2", layer, which, 3)
        B2 = self.load_mod("xB2", layer, which, 4)
        t = self.alloc_norm_tmp("x")
        mixg = [S.alloc("xmix%d" % i, [8, 512], BF16) for i in range(2)]
        xg = [S.alloc("xx%d" % i, [4, D], F32) for i in range(2)]
        aT = [S.alloc("xaT%d" % i, [8, 512], BF16) for i in range(2)]
        ytmp = [S.alloc("xyt%d" % i, [512], F32) for i in range(2)]
        PS = self.PS
        if router is not None:
            RWb = S.alloc("xRW", [NEXP, D], F32)
            rw = router['w']
            for e_ in range(NEXP):
                self.load(RWb, RWb.ap[:, e_, :], rw[e_:e_ + 1, :].partition_broadcast(128))
            rj = S.alloc("xrj", [D], F32)
            lg = [S.alloc("xlg%d" % i, [64], F32) for i in range(2)]
        ngr = (nq + 511) // 512
        groups = [(g * 512, min(512, nq - g * 512)) for g in range(ngr)]

        def issue_load(gi):
            t0, G = groups[gi]
            nt = G // 128
            self.load(mixg[gi % 2], mixg[gi % 2].ap[:, :, 0:G], OT[:, :, t0:t0 + G].rearrange("c p n -> p c n"), src=ot_buf)
            self.load(xg[gi % 2], xg[gi % 2].ap[:, 0:nt, :], res_src[t0:t0 + G, :].rearrange("(t p) d -> p t d", p=128), src=res_buf)
        issue_load(0)
        kk = 0
        for gi, (t0, G) in enumerate(groups):
            if gi + 1 < len(groups):
                issue_load(gi + 1)
            nt = G // 128
            mg, xs, aTs = mixg[gi % 2], xg[gi % 2], aT[gi % 2]
            for i in range(nt):
                for hf in range(2):
                    yp = PS[2 + kk % 2]
                    yt = ytmp[kk % 2]
                    kk += 1
                    self.mm(yp, yp.ap[:, :], [mg.ap[:, c, i * 128:(i + 1) * 128] for c in range(8)],
                            [Wo.ap[:, c, hf * 512:(hf + 1) * 512] for c in range(8)], [mg, Wo])
                    self.tt('vector', yt, yt.ap[:], yp, yp.ap[:], G1, G1.ap[:, hf * 512:(hf + 1) * 512], ALU.mult)
                    self.tt('gpsimd', xs, xs.ap[:, i, hf * 512:(hf + 1) * 512], yt, yt.ap[:], xs, xs.ap[:, i, hf * 512:(hf + 1) * 512], ALU.add)
            self.store(h1_buf, H1[t0:t0 + G, :].rearrange("(t p) d -> p t d", p=128), xs, xs.ap[:, 0:nt, :])
            afs = self.run_norm(t, [(xs, xs.ap[:, i, :]) for i in range(nt)], A2, B2,
                                [(aTs, aTs.ap[:, :, i * 128:(i + 1) * 128]) for i in range(nt)],
                                want_f32=(router is not None), tr_banks=(0, 1)) if router is None else None
            if router is not None:
                i = 0
                for af in self.norm_mod_T(t, [(xs, xs.ap[:, i2, :]) for i2 in range(nt)], A2, B2,
                                          [(aTs, aTs.ap[:, :, i2 * 128:(i2 + 1) * 128]) for i2 in range(nt)], want_f32=True):
                    self.router_tile(af, RWb, rj, lg[i % 2], router['wts'], (t0 // 128) + i)
                    i += 1
            self.store(a2t_buf, A2T[:, :, t0:t0 + G].rearrange("c p n -> p c n"), aTs, aTs.ap[:, :, 0:G])
        S.barrier()
        S.release(m0)

    def router_tile(self, af, RWb, rj, lg, wts, tile):
        for e_ in range(NEXP):
            self.stt(rj, rj.ap[:], af, af.ap[:], 1.0, RWb, RWb.ap[:, e_, :], ALU.mult, ALU.mult, writes=[lg], accum_out=lg.ap[:, e_:e_ + 1])
        L_ = lg.ap
        V = 'vector'
        self.S.op(V, [lg], [lg], lambda e: e.tensor_reduce(out=L_[:, 8:9], in_=L_[:, 0:8], axis=AX.X, op=ALU.max))
        self.ts(V, lg, L_[:, 9:10], lg, L_[:, 8:9], -1.0, None, ALU.mult)
        self.ts(V, lg, L_[:, 10:18], lg, L_[:, 0:8], L_[:, 8:9], None, ALU.is_ge)
        self.stt(lg, L_[:, 18:26], lg, L_[:, 10:18], -1e30, lg, L_[:, 0:8], ALU.mult, ALU.add)
        self.S.op(V, [lg], [lg], lambda e: e.tensor_reduce(out=L_[:, 26:27], in_=L_[:, 18:26], axis=AX.X, op=ALU.max))
        self.ts(V, lg, L_[:, 10:18], lg, L_[:, 0:8], L_[:, 26:27], None, ALU.is_ge)
        self.act(lg, L_[:, 27:35], lg, L_[:, 0:8], AF.Exp, bias=L_[:, 9:10], scale=1.0)
        self.tt(V, lg, L_[:, 27:35], lg, L_[:, 27:35], lg, L_[:, 10:18], ALU.mult)
        self.S.op(V, [lg], [lg], lambda e: e.tensor_reduce(out=L_[:, 35:36], in_=L_[:, 27:35], axis=AX.X, op=ALU.add))
        self.S.op(V, [lg], [lg], lambda e: e.reciprocal(out=L_[:, 36:37], in_=L_[:, 35:36]))
        self.ts(V, wts, wts.ap[:, tile, :], lg, L_[:, 27:35], L_[:, 36:37], None, ALU.mult)

    def y_phase(self, tag, layer, which, nq, A2T, a2t_buf, H1, h1_buf, wg, wu, wd, nexp, wts, out_dram, out_buf, final_g=None, T=1024):
        S = self.S
        m0 = S.mark()
        G2 = self.load_mod("yG2", layer, which, 5)
        if final_g is not None:
            FG = S.alloc("yFG", [D], F32)
            self.load(FG, FG.ap[:], final_g.rearrange("(o d) -> o d", o=1).partition_broadcast(128))
        ntmax = T // 128
        aT = S.alloc("yaT", [8, T], BF16)
        acc = S.alloc("yacc", [ntmax, D], F32)
        hT = S.alloc("yhT", [FC, T], BF16)
        NB = 3
        wgb = [S.alloc("ywg%d" % i, [8, 128], BF16) for i in range(NB)]
        wub = [S.alloc("ywu%d" % i, [8, 128], BF16) for i in range(NB)]
        wdh = [S.alloc("ywd%d" % i, [11, D], BF16) for i in range(2)]
        sg = [S.alloc("ysg%d" % i, [512], F32) for i in range(2)]
        yt = [S.alloc("yyt%d" % i, [512], F32) for i in range(2)]
        fss = S.alloc("yfss", [2 * ntmax], F32)
        fjunk = S.alloc("yfj", [D], BF16)
        PS = self.PS
        ngr = (nq + T - 1) // T
        kw = 0
        kd = 0
        kk = 0
        for gi in range(ngr):
            t0 = gi * T
            G = min(T, nq - t0)
            nt = G // 128
            self.load(aT, aT.ap[:, :, 0:G], A2T[:, :, t0:t0 + G].rearrange("c p n -> p c n"), src=a2t_buf)
            self.load(acc, acc.ap[:, 0:nt, :], H1[t0:t0 + G, :].rearrange("(t p) d -> p t d", p=128), src=h1_buf)
            for e_ in range(nexp):
                wge = wg[e_] if nexp > 1 else wg
                wue = wu[e_] if nexp > 1 else wu
                wde = wd[e_] if nexp > 1 else wd
                for j in range(FC):
                    a, b = wgb[kw % NB], wub[kw % NB]
                    kw += 1
                    self.load(a, a.ap[:], wge[:, j * 128:(j + 1) * 128].rearrange("(c p) n -> p c n", p=128), q='gpsimd')
                    self.load(b, b.ap[:], wue[:, j * 128:(j + 1) * 128].rearrange("(c p) n -> p c n", p=128), q='gpsimd')
                    for q0 in range(0, G, 512):
                        Gq = min(512, G - q0)
                        gp, up = PS[kk % 2], PS[2 + kk % 2]
                        s_ = sg[kk % 2]
                        kk += 1
                        rhs = [aT.ap[:, c, q0:q0 + Gq] for c in range(8)]
                        self.mm(gp, gp.ap[:, 0:Gq], [a.ap[:, c, :] for c in range(8)], rhs, [a, aT])
                        self.mm(up, up.ap[:, 0:Gq], [b.ap[:, c, :] for c in range(8)], rhs, [b, aT])
                        self.act(s_, s_.ap[:, 0:Gq], gp, gp.ap[:, 0:Gq], AF.Silu)
                        self.tt('vector', hT, hT.ap[:, j, q0:q0 + Gq], s_, s_.ap[:, 0:Gq], up, up.ap[:, 0:Gq], ALU.mult)
                for hh in range(2):
                    j0, j1 = hh * 11, (hh + 1) * 11
                    self.load(wdh[hh], wdh[hh].ap[:, :, :], wde[j0 * 128:j1 * 128, :].rearrange("(j p) n -> p j n", p=128), q='gpsimd')
                for i in range(nt):
                    dp = [PS[4 + 2 * (kd % 2)], PS[5 + 2 * (kd % 2)]]
                    kd += 1
                    for hf in range(2):
                        for hh in range(2):
                            self.mm(dp[hf], dp[hf].ap[:, :], [hT.ap[:, hh * 11 + jj, i * 128:(i + 1) * 128] for jj in range(11)],
                                    [wdh[hh].ap[:, jj, hf * 512:(hf + 1) * 512] for jj in range(11)],
                                    [hT, wdh[hh]], start=(hh == 0), stop=(hh == 1))
                    for hf in range(2):
                        y_ = yt[hf]
                        if wts is not None:
                            self.stt(y_, y_.ap[:], dp[hf], dp[hf].ap[:], wts.ap[:, t0 // 128 + i, e_:e_ + 1], G2, G2.ap[:, hf * 512:(hf + 1) * 512],
                                     ALU.mult, ALU.mult, reads=[wts])
                        else:
                            self.tt('vector', y_, y_.ap[:], dp[hf], dp[hf].ap[:], G2, G2.ap[:, hf * 512:(hf + 1) * 512], ALU.mult)
                        self.tt('gpsimd', acc, acc.ap[:, i, hf * 512:(hf + 1) * 512], y_, y_.ap[:], acc, acc.ap[:, i, hf * 512:(hf + 1) * 512], ALU.add)
            if final_g is not None:
                for i in range(nt):
                    self.S.op('scalar', [acc], [fjunk, fss],
                              lambda e, i=i: e.activation(out=fjunk.ap[:], in_=acc.ap[:, i, :], func=AF.Square, accum_out=fss.ap[:, i:i + 1]))
                self.act(fss, fss.ap[:, ntmax:ntmax + nt], fss, fss.ap[:, 0:nt], AF.Sqrt, reads=[self.epsc], scale=1.0 / D, bias=self.epsc.ap[:])
                self.S.op('vector', [fss], [fss], lambda e: e.reciprocal(out=fss.ap[:, ntmax:ntmax + nt], in_=fss.ap[:, ntmax:ntmax + nt]))
                for i in range(nt):
                    self.stt(acc, acc.ap[:, i, :], acc, acc.ap[:, i, :], fss.ap[:, ntmax + i:ntmax + i + 1], FG, FG.ap[:], ALU.mult, ALU.mult, reads=[fss])
            self.store(out_buf, out_dram[t0:t0 + G, :].rearrange("(t p) d -> p t d", p=128), acc, acc.ap[:, 0:nt, :])
        S.barrier()
        S.release(m0)


    def l1_latents(self, tag, src, src_buf, n, which, ropeT, CQ, cq_buf, LAT, lat_buf, want_q):
        S = self.S
        m0 = S.mark()
        w_in1 = self.inp_once("odd_w_in", [D, 832])
        qg = self.inp_once("q_norm_g", [512])
        kvg = self.inp_once("kv_norm_g", [256])
        Win = S.alloc("l1Win", [8, 832], BF16)
        self.load(Win, Win.ap[:], w_in1.rearrange("(c p) n -> p c n", p=128), q='gpsimd')
        gq = S.alloc("l1gq", [4], F32)
        gkv = S.alloc("l1gkv", [2], F32)
        self.load(gq, gq.ap[:], qg.rearrange("(c p) -> p c", p=128), allow_slow_non_contiguous=True)
        self.load(gkv, gkv.ap[:], kvg.rearrange("(c p) -> p c", p=128), allow_slow_non_contiguous=True)
        A = self.load_mod("l1A", 1, which, 0)
        B = self.load_mod("l1B", 1, which, 1)
        t = self.alloc_norm_tmp("l1")
        xg = [S.alloc("l1x%d" % i, [4, D], F32) for i in range(2)]
        aT = [S.alloc("l1aT%d" % i, [8, 512], BF16) for i in range(2)]
        cs = [S.alloc("l1cs%d" % i, [2, 512], F32) for i in range(2)]
        zz = [S.alloc("l1z%d" % i, [512], BF16) for i in range(6)]
        sq = [S.alloc("l1sq%d" % i, [512], BF16) for i in range(2)]
        rs = [S.alloc("l1rs%d" % i, [512], F32) for i in range(2)]
        st = [S.alloc("l1st%d" % i, [512], BF16) for i in range(4)]
        kz = S.alloc("l1kz", [512], BF16)
        t1 = S.alloc("l1t1", [512], F32)
        t2 = S.alloc("l1t2", [512], F32)
        PS = self.PS
        ngr = (n + 511) // 512
        groups = [(g * 512, min(512, n - g * 512)) for g in range(ngr)]

        def issue_load(gi):
            t0, G = groups[gi]
            self.load(xg[gi % 2], xg[gi % 2].ap[:, 0:G // 128, :], src[t0:t0 + G, :].rearrange("(t p) d -> p t d", p=128), src=src_buf)
            if ropeT is not None:
                self.load(cs[gi % 2], cs[gi % 2].ap[:, :, 0:G], ropeT[:, :, t0:t0 + G].rearrange("a p n -> p a n"))
        issue_load(0)
        kk = 0
        ks = 0
        for gi, (t0, G) in enumerate(groups):
            if gi + 1 < len(groups):
                issue_load(gi + 1)
            nt = G // 128
            xs, aTs, c_ = xg[gi % 2], aT[gi % 2], cs[gi % 2]
            self.run_norm(t, [(xs, xs.ap[:, i, :]) for i in range(nt)], A, B,
                          [(aTs, aTs.ap[:, :, i * 128:(i + 1) * 128]) for i in range(nt)])
            parts = []
            if want_q:
                parts.append((0, 4, 512.0, gq, CQ, cq_buf, 0, 4))
            parts.append((512, 2, 256.0, gkv, LAT, lat_buf, 4, 5))
            for (c0, nch, dim, gcol, dst, dst_buf, zoff, sbank) in parts:
                ssq = PS[sbank]
                for c in range(nch):
                    zp = PS[2 + kk % 2]
                    kk += 1
                    z = zz[zoff + c]
                    s_ = sq[c % 2]
                    self.mm(zp, zp.ap[:, 0:G], [Win.ap[:, k, c0 + c * 128:c0 + (c + 1) * 128] for k in range(8)],
                            [aTs.ap[:, k, 0:G] for k in range(8)], [Win, aTs])
                    self.cp('scalar', z, z.ap[:, 0:G], zp, zp.ap[:, 0:G])
                    self.act(s_, s_.ap[:, 0:G], zp, zp.ap[:, 0:G], AF.Square)
                    self.mm(ssq, ssq.ap[:, 0:G], [self.onesb.ap[:]], [s_.ap[:, 0:G]], [self.onesb, s_], start=(c == 0), stop=(c == nch - 1))
                r = rs[0 if zoff == 0 else 1]
                self.act(r, r.ap[:, 0:G], ssq, ssq.ap[:, 0:G], AF.Sqrt, reads=[self.epsc], scale=1.0 / dim, bias=self.epsc.ap[:])
                self.S.op('vector', [r], [r], lambda e, r=r, G=G: e.reciprocal(out=r.ap[:, 0:G], in_=r.ap[:, 0:G]))
                for c in range(nch):
                    z = zz[zoff + c]
                    o = st[ks % 4]
                    ks += 1
                    self.stt(o, o.ap[:, 0:G], z, z.ap[:, 0:G], gcol.ap[:, c:c + 1], r, r.ap[:, 0:G], ALU.mult, ALU.mult, reads=[gcol])
                    self.store(dst_buf, dst(c, 0, 128, t0, G), o, o.ap[:, 0:G])
            zp = PS[2 + kk % 2]
            kk += 1
            self.mm(zp, zp.ap[0:64, 0:G], [Win.ap[:, k, 768:832] for k in range(8)], [aTs.ap[:, k, 0:G] for k in range(8)], [Win, aTs])
            o = st[ks % 4]
            ks += 1
            if ropeT is not None:
                self.rope_tile(64, G, zp, kz, PS[6], t1, t2, c_, c_.ap[0:64, 0, 0:G], c_, c_.ap[0:64, 1, 0:G], o, o.ap[0:64, 0:G])
            else:
                self.cp('scalar', o, o.ap[0:64, 0:G], zp, zp.ap[0:64, 0:G])
            self.store(lat_buf, LAT(2, 0, 64, t0, G), o, o.ap[0:64, 0:G])
        S.barrier()
        S.release(m0)

    def l1_kv_build(self, latsrc):
        S = self.S
        m0 = S.mark()
        NK, NT = self.NK, self.NT
        w_ukv = self.inp_once("w_ukv", [256, 2048])
        self.KT1 = self.scratch("KT1", [8, 128, NK], BF16)
        self.V1 = self.scratch("V1", [8, 128, NT, 128], BF16)
        self.kt1_buf = S.dram_buf("KT1")
        self.v1_buf = S.dram_buf("V1")
        Wkn = S.alloc("l1Wkn", [2, 8, 128], BF16, multi=True)
        Wvv = S.alloc("l1Wvv", [2, 8, 128], BF16, multi=True)
        wv = w_ukv.rearrange("(c p) (h x) -> c p h x", p=128, x=256)
        for c in range(2):
            self.load(Wkn, Wkn.ap[:, c, :, :], wv[c, :, :, 0:128], q='gpsimd')
            self.load(Wvv, Wvv.ap[:, c, :, :], wv[c, :, :, 128:256], q='gpsimd')
        latg = [S.alloc("l1lg%d" % i, [2, 512], BF16) for i in range(2)]
        kst = [S.alloc("l1ks%d" % i, [512], BF16) for i in range(4)]
        vst = [S.alloc("l1vs%d" % i, [8, 4, 128], BF16) for i in range(2)]
        PS = self.PS
        groups = [(k0, min(512, NK - k0)) for k0 in range(0, NK, 512)]

        def issue_load(gi):
            k0, G = groups[gi]
            ap_, b_ = latsrc['lat2'](k0, G)
            self.load(latg[gi % 2], latg[gi % 2].ap[:, :, 0:G], ap_, src=b_)
        issue_load(0)
        kk = 0
        for gi, (k0, G) in enumerate(groups):
            if gi + 1 < len(groups):
                issue_load(gi + 1)
            nt = G // 128
            lg = latg[gi % 2]
            for h in range(8):
                zp = PS[kk % 4]
                ks = kst[kk % 4]
                kk += 1
                self.mm(zp, zp.ap[:, 0:G], [Wkn.ap[:, c, h, :] for c in range(2)], [lg.ap[:, c, 0:G] for c in range(2)], [Wkn, lg])
                self.cp('scalar' if h % 2 else 'vector', ks, ks.ap[:, 0:G], zp, zp.ap[:, 0:G])
                self.store(self.kt1_buf, self.KT1[h, :, k0:k0 + G], ks, ks.ap[:, 0:G])
            vs = vst[gi % 2]
            for i in range(nt):
                for hf in range(2):
                    vp = PS[4 + kk % 4]
                    kk += 1
                    self.mm(vp, vp.ap[:, :], [lg.ap[:, c, i * 128:(i + 1) * 128] for c in range(2)],
                            [Wvv.ap[:, c, hf * 4:(hf + 1) * 4, :] for c in range(2)], [Wvv, lg])
                    self.cp('vector' if hf else 'scalar', vs, vs.ap[:, hf * 4:(hf + 1) * 4, i, :], vp, vp.ap[:, :].rearrange("p (h d) -> p h d", h=4))
            tile0 = k0 // 128
            self.store(self.v1_buf, self.V1[:, :, tile0:tile0 + nt, :].rearrange("h p t d -> p h t d"), vs, vs.ap[:, :, 0:nt, :])
        S.barrier()
        S.release(m0)

    def l1_q_build(self, nq, CQ, cq_buf, ropeQ):
        S = self.S
        m0 = S.mark()
        w_uq = self.inp_once("w_uq", [512, 1536])
        self.QN1 = self.scratch("QN1", [8, 128, nq], BF16)
        self.QR1 = self.scratch("QR1", [8, 64, nq], BF16)
        self.qn1_buf = S.dram_buf("QN1")
        self.qr1_buf = S.dram_buf("QR1")
        Wn = S.alloc("l1Wn", [4, 8, 128], BF16, multi=True)
        Wr = S.alloc("l1Wr", [4, 8, 64], BF16, multi=True)
        wv = w_uq.rearrange("(c p) (h x) -> c p h x", p=128, x=192)
        for c in range(4):
            self.load(Wn, Wn.ap[:, c, :, :], wv[c, :, :, 0:128], q='gpsimd')
            self.load(Wr, Wr.ap[:, c, :, :], wv[c, :, :, 128:192], q='gpsimd')
        cqg = [S.alloc("l1cq%d" % i, [4, 512], BF16) for i in range(2)]
        cs = [S.alloc("l1qcs%d" % i, [2, 512], F32) for i in range(2)]
        st = [S.alloc("l1qst%d" % i, [512], BF16) for i in range(4)]
        kz = S.alloc("l1qkz", [512], BF16)
        t1 = S.alloc("l1qt1", [512], F32)
        t2 = S.alloc("l1qt2", [512], F32)
        PS = self.PS
        groups = [(q0, min(512, nq - q0)) for q0 in range(0, nq, 512)]

        def issue_load(gi):
            q0, G = groups[gi]
            self.load(cqg[gi % 2], cqg[gi % 2].ap[:, :, 0:G], CQ[:, :, q0:q0 + G].rearrange("c p n -> p c n"), src=cq_buf)
            self.load(cs[gi % 2], cs[gi % 2].ap[:, :, 0:G], ropeQ[:, :, q0:q0 + G].rearrange("a p n -> p a n"))
        issue_load(0)
        kk = 0
        for gi, (q0, G) in enumerate(groups):
            if gi + 1 < len(groups):
                issue_load(gi + 1)
            cg, c_ = cqg[gi % 2], cs[gi % 2]
            for h in range(8):
                zp = PS[kk % 2]
                zr = PS[2 + kk % 2]
                rp = PS[4 + kk % 2]
                o1 = st[(2 * kk) % 4]
                o2 = st[(2 * kk + 1) % 4]
                kk += 1
                self.mm(zp, zp.ap[:, 0:G], [Wn.ap[:, c, h, :] for c in range(4)], [cg.ap[:, c, 0:G] for c in range(4)], [Wn, cg])
                self.cp('vector', o1, o1.ap[:, 0:G], zp, zp.ap[:, 0:G])
                self.store(self.qn1_buf, self.QN1[h, :, q0:q0 + G], o1, o1.ap[:, 0:G])
                self.mm(zr, zr.ap[0:64, 0:G], [Wr.ap[:, c, h, :] for c in range(4)], [cg.ap[:, c, 0:G] for c in range(4)], [Wr, cg])
                self.rope_tile(64, G, zr, kz, rp, t1, t2, c_, c_.ap[0:64, 0, 0:G], c_, c_.ap[0:64, 1, 0:G], o2, o2.ap[0:64, 0:G])
                self.store(self.qr1_buf, self.QR1[h, :, q0:q0 + G], o2, o2.ap[0:64, 0:G])
        S.barrier()
        S.release(m0)

    def l1_attention(self, nq, latsrc, OT, ot_buf):
        S = self.S
        m0 = S.mark()
        NK = self.NK
        KR = S.alloc("l1KR", [NK], BF16, multi=True)
        for (c0, c1, ap_, b_) in latsrc['kr']:
            self.load(KR, KR.ap[0:64, c0:c1], ap_, src=b_)
        QNb = [S.alloc("l1QN%d" % i, [nq], BF16) for i in range(2)]
        QRb = [S.alloc("l1QR%d" % i, [nq], BF16) for i in range(2)]
        rr = [S.alloc("l1r%d" % i, [512], F32) for i in range(2)]
        oo = [S.alloc("l1o%d" % i, [512], BF16) for i in range(2)]
        ectr = [0]

        def epilogue(h, qc, G, O, Lq):
            r = rr[ectr[0] % 2]
            o = oo[ectr[0] % 2]
            ectr[0] += 1
            self.S.op('vector', [Lq[0]], [r], lambda e: e.reciprocal(out=r.ap[:, 0:G], in_=Lq[0].ap[:, 0:G]))
            self.tt('vector', o, o.ap[:, 0:G], O[0], O[0].ap[:, 0:G], r, r.ap[:, 0:G], ALU.mult)
            self.store(ot_buf, OT[h, :, qc * 512:qc * 512 + G], o, o.ap[:, 0:G])

        def qprov(h):
            qn, qr = QNb[h % 2], QRb[h % 2]
            self.load(qn, qn.ap[:], self.QN1[h], src=self.qn1_buf)
            self.load(qr, qr.ap[0:64, :], self.QR1[h], src=self.qr1_buf)

            def f(qc):
                G = min(512, nq - qc * 512)
                q0 = qc * 512
                return [[('main', qn, qn.ap[:, q0:q0 + G], (0, 128)), ('rope', qr, qr.ap[0:64, q0:q0 + G], (0, 64))]]
            return f
        cfg = dict(nh=8, nq=nq, key0=0, nkeys=NK, maps=1, scale=192 ** -0.5,
                   KT=lambda h: self.KT1[h], V=lambda h: self.V1[h], kbuf=self.kt1_buf, vbuf=self.v1_buf,
                   qprov=qprov, epilogue=epilogue, KR=KR)
        self.attention(cfg)
        S.barrier()
        S.release(m0)

    def build_A(self, fused=False):
        S, L, NQ, C = self.S, self.L, self.NQ, self.C
        self.setup_consts()
        self.mod_prologue([0, 1])
        self.l0_kv_build()
        xo = self.inp("xo", [NQ, D])
        xh = self.inp("xh", [128, D])
        xhc = self.inp("xhc", [128, D])
        hmask = self.inp("hmask", [128, 16])
        zmask = self.inp("zmask", [128, 16])
        icnt = self.inp("icnt", [128, 2, NQ])
        icntc = self.inp("icntc", [128, 2, C])
        ropeQ = self.inp("ropeQ", [2, 128, NQ])
        ctxb = self.nc_inputs_ap("ctxb")
        w_out0 = self.inp("even_w_out", [D, D])
        wg0 = self.inp("ffn_w_gate", [D, FF])
        wu0 = self.inp("ffn_w_up", [D, FF])
        wd0 = self.inp("ffn_w_down", [FF, D])
        h2 = self.scratch("h2", [NQ, D], F32) if fused else self.outp("h2", [NQ, D])
        hc2 = self.scratch("hc2", [C, D], F32)
        paths = [
            ("lat", xo, NQ, xh, hmask, 0, ropeQ, icnt, 0, self.NK, h2, "h2"),
            ("ctx", ctxb, C, xhc, zmask, 1, None, icntc, L, C, hc2, "hc2"),
        ]
        for (tag, xq, nq, xhal, hm, which, rq, ic, key0, nkeys, hout, hname) in paths:
            QTs = self.scratch("QTs_" + tag, [6, 128, nq], BF16)
            OT = self.scratch("OT_" + tag, [8, 128, nq], BF16)
            H1 = self.scratch("H1_" + tag, [nq, D], F32)
            A2T = self.scratch("A2T_" + tag, [8, 128, nq], BF16)
            qt_buf, ot_buf = S.dram_buf("QTs_" + tag), S.dram_buf("OT_" + tag)
            h1_buf, a2t_buf = S.dram_buf("H1_" + tag), S.dram_buf("A2T_" + tag)
            mp = S.mark()
            pext = S.alloc("pext", [2, nq + 16], F32)
            self.l0_q_build(tag, xq, nq, xhal, hm, which, rq, QTs, qt_buf, pext)
            self.l0_pool(nq, pext, ic, OT, ot_buf)
            S.barrier()
            S.release(mp)
            self.l0_attention(nq, key0, nkeys, QTs, qt_buf, OT, ot_buf, 0.2)
            self.x_phase(tag, 0, which, nq, OT, ot_buf, xq, None, w_out0, H1, h1_buf, A2T, a2t_buf)
            self.y_phase(tag, 0, which, nq, A2T, a2t_buf, H1, h1_buf, wg0, wu0, wd0, 1, None, hout, S.dram_buf(hname))
        if fused:
            cq = self.scratch("cq", [4, 128, NQ], BF16)
            nlch = NQ // 512
            lat_loc = [self.scratch("lat_loc%d" % j, [384, 512], BF16) for j in range(nlch)]
            latc = self.scratch("latc", [3, 128, C], BF16)
        else:
            cq = self.outp("cq", [4, 128, NQ], BF16)
            lat = self.outp("lat", [3, 128, NQ], BF16)
            latc = self.outp("latc", [3, 128, C], BF16)
        cqf = lambda c, r0, r1, t0, G: cq[c, r0:r1, t0:t0 + G]
        latcf = lambda c, r0, r1, t0, G: latc[c, r0:r1, t0:t0 + G]
        if fused:
            latf = lambda c, r0, r1, t0, G: lat_loc[t0 // 512].rearrange("(c p) n -> c p n", c=3)[c, r0:r1, 0:G]
        else:
            latf = lambda c, r0, r1, t0, G: lat[c, r0:r1, t0:t0 + G]
        self.l1_latents("lat", h2, S.dram_buf("h2"), NQ, 0, ropeQ, cqf, S.dram_buf("cq"), latf, S.dram_buf("lat"), True)
        self.l1_latents("ctx", hc2, S.dram_buf("hc2"), C, 1, None, None, None, latcf, S.dram_buf("latc"), False)
        if not fused:
            return S.finish()
        nqc = L // NQ
        lat_g = [self.scratch("lat_g%d" % j, [nqc * 384, 512], BF16) for j in range(nlch)]
        bg = S.dram_buf("lat_g")
        groups = [list(range(b * nqc, (b + 1) * nqc)) for b in range(2)]
        for j in range(nlch):
            S.dma_custom('gpsimd', lambda e, j=j: e.collective_compute("AllGather", ALU.bypass, replica_groups=groups,
                                                                        ins=[lat_loc[j].opt()], outs=[lat_g[j].opt()]),
                         [S.dram_buf("lat")], [bg], inc=1)
        Gv = [g_.rearrange("(r c p) n -> r c p n", r=nqc, c=3) for g_ in lat_g]
        bc = S.dram_buf("latc")

        def lat2(k0, G):
            if k0 < L:
                r, n0 = k0 // NQ, k0 % NQ
                return Gv[n0 // 512][r, 0:2, :, 0:G].rearrange("c p n -> p c n"), bg
            return latc[0:2, :, k0 - L:k0 - L + G].rearrange("c p n -> p c n"), bc
        kr = [(r * NQ + j * 512, r * NQ + (j + 1) * 512, Gv[j][r, 2, 0:64, :], bg) for r in range(nqc) for j in range(nlch)]
        kr.append((L, L + C, latc[2, 0:64, :], bc))
        latsrc = dict(lat2=lat2, kr=kr)
        w_out1 = self.inp("odd_w_out", [D, D])
        rwT = self.inp("router_wT", [NEXP, D])
        wg1 = self.inp("moe_w_gate", [NEXP, D, FF])
        wu1 = self.inp("moe_w_up", [NEXP, D, FF])
        wd1 = self.inp("moe_w_down", [NEXP, FF, D])
        fg = self.inp("final_norm_g", [D])
        out = self.outp("out", [NQ, D])
        S.rotate()
        self.l1_kv_build(latsrc)
        self.l1_q_build(NQ, cq, S.dram_buf("cq"), ropeQ)
        OT = self.scratch("OT1", [8, 128, NQ], BF16)
        H1 = self.scratch("H1_1", [NQ, D], F32)
        A2T = self.scratch("A2T_1", [8, 128, NQ], BF16)
        ot_buf, h1_buf, a2t_buf = S.dram_buf("OT1"), S.dram_buf("H1_1"), S.dram_buf("A2T_1")
        self.l1_attention(NQ, latsrc, OT, ot_buf)
        mw = S.mark()
        wts = S.alloc("wts", [NQ // 128, NEXP], F32)
        self.x_phase("l1", 1, 0, NQ, OT, ot_buf, h2, S.dram_buf("h2"), w_out1, H1, h1_buf, A2T, a2t_buf, router=dict(w=rwT, wts=wts))
        self.y_phase("l1", 1, 0, NQ, A2T, a2t_buf, H1, h1_buf, wg1, wu1, wd1, NEXP, wts, out, S.dram_buf("out"), final_g=fg)
        S.barrier()
        S.release(mw)
        return S.finish()

    def nc_inputs_ap(self, name):
        return self._inp_aps[name]

    def build_B(self):
        S, L, NQ, C = self.S, self.L, self.NQ, self.C
        self.setup_consts()
        self.mod_prologue([1])
        h2 = self.inp("h2", [NQ, D])
        cq = self.inp("cq", [4, 128, NQ], BF16)
        latall = self.inp("latall", [3, 128, self.NK], BF16)
        ropeQ = self.inp("ropeQ", [2, 128, NQ])
        w_out1 = self.inp("odd_w_out", [D, D])
        rwT = self.inp("router_wT", [NEXP, D])
        wg1 = self.inp("moe_w_gate", [NEXP, D, FF])
        wu1 = self.inp("moe_w_up", [NEXP, D, FF])
        wd1 = self.inp("moe_w_down", [NEXP, FF, D])
        fg = self.inp("final_norm_g", [D])
        out = self.outp("out", [NQ, D])
        latsrc = dict(lat2=lambda k0, G: (latall[0:2, :, k0:k0 + G].rearrange("c p n -> p c n"), None),
                      kr=[(0, self.NK, latall[2, 0:64, :], None)])
        self.l1_kv_build(latsrc)
        self.l1_q_build(NQ, cq, None, ropeQ)
        OT = self.scratch("OT1", [8, 128, NQ], BF16)
        H1 = self.scratch("H1_1", [NQ, D], F32)
        A2T = self.scratch("A2T_1", [8, 128, NQ], BF16)
        ot_buf, h1_buf, a2t_buf = S.dram_buf("OT1"), S.dram_buf("H1_1"), S.dram_buf("A2T_1")
        self.l1_attention(NQ, latsrc, OT, ot_buf)
        mw = S.mark()
        wts = S.alloc("wts", [NQ // 128, NEXP], F32)
        self.x_phase("l1", 1, 0, NQ, OT, ot_buf, h2, None, w_out1, H1, h1_buf, A2T, a2t_buf, router=dict(w=rwT, wts=wts))
        self.y_phase("l1", 1, 0, NQ, A2T, a2t_buf, H1, h1_buf, wg1, wu1, wd1, NEXP, wts, out, S.dram_buf("out"), final_g=fg)
        S.barrier()
        S.release(mw)
        return S.finish()


def _rope_tables(L):
    t = np.arange(L)
    row = (t // GRID_W).astype(np.float32)
    col = (t % GRID_W).astype(np.float32)
    inv = (10000.0 ** (-np.arange(16, dtype=np.float32) / 16)).astype(np.float32)
    ar = row[:, None] * inv[None, :]
    ac = col[:, None] * inv[None, :]
    cos = np.concatenate([np.cos(ar), np.cos(ar), np.cos(ac), np.cos(ac)], axis=-1).astype(np.float32)
    sin = np.concatenate([np.sin(ar), np.sin(ar), np.sin(ac), np.sin(ac)], axis=-1).astype(np.float32)
    cosT = np.concatenate([cos.T, cos.T], axis=0)
    sinT = np.concatenate([sin.T, sin.T], axis=0)
    return np.ascontiguousarray(np.stack([cosT, sinT], axis=0))


def _rmat():
    R = np.zeros((128, 128), np.float32)
    for blk in range(2):
        o = blk * 64
        for i in range(16):
            R[o + 16 + i, o + i] = -1.0
            R[o + i, o + 16 + i] = 1.0
            R[o + 48 + i, o + 32 + i] = -1.0
            R[o + 32 + i, o + 48 + i] = 1.0
    return R


def _icnt(start, n, l):
    out = np.zeros((128, 2, n), np.float32)
    t = np.arange(start, start + n)
    for g, w in enumerate(POOL_WINDOWS):
        lo = np.clip(t - w // 2, 0, l)
        hi = np.clip(t + w // 2, 0, l)
        ic = (1.0 / (hi - lo)).astype(np.float32)
        j, hf = g // 2, g % 2
        out[hf * 64:(hf + 1) * 64, j, :] = ic[None, :]
    return out


_PROG_CACHE = {}


def _get_prog(L, NQ, C, stage):
    key = (L, NQ, C, stage)
    if key not in _PROG_CACHE:
        p = Prog(L, NQ, C, stage)
        p._inp_aps = {}
        orig_inp = p.inp

        def inp(name, shape, dtype=F32):
            ap = orig_inp(name, shape, dtype)
            p._inp_aps[name] = ap
            return ap
        p.inp = inp
        n = p.build_A(fused=True) if stage == 'AB' else (p.build_A() if stage == 'A' else p.build_B())
        p.n_instr = n
        _PROG_CACHE[key] = p
    return _PROG_CACHE[key]


def _run_fused(L, NQ, C, inputs):
    B = inputs["x"].shape[0]
    nqc = L // NQ
    ncores = B * nqc
    f32 = lambda a: np.ascontiguousarray(np.asarray(a, dtype=np.float32))
    x, c, ctx, c_ctx = f32(inputs["x"]), f32(inputs["c"]), f32(inputs["ctx"]), f32(inputs["c_ctx"])
    rope = _rope_tables(L)
    lam = np.stack([f32(inputs["lambda_q1"])[0], f32(inputs["lambda_k1"])[0], f32(inputs["lambda_q2"])[0], f32(inputs["lambda_k2"])[0]], axis=0)
    common = {
        "c_ident": np.eye(128, dtype=np.float32), "c_rmat": _rmat(),
        "mod_w": f32(inputs["mod_w"]), "mod_b": f32(inputs["mod_b"]),
        "norm1_g": f32(inputs["norm1_g"]), "norm2_g": f32(inputs["norm2_g"]),
        "even_w_in": f32(inputs["even_w_in"])[0], "pool_w": f32(inputs["pool_w"])[0], "pool_scale": f32(inputs["pool_scale"])[0],
        "lam": np.ascontiguousarray(lam), "even_w_out": f32(inputs["even_w_out"])[0],
        "ffn_w_gate": f32(inputs["ffn_w_gate"])[0], "ffn_w_up": f32(inputs["ffn_w_up"])[0], "ffn_w_down": f32(inputs["ffn_w_down"])[0],
        "odd_w_in": f32(inputs["odd_w_in"])[0], "q_norm_g": f32(inputs["q_norm_g"])[0], "kv_norm_g": f32(inputs["kv_norm_g"])[0],
        "ropeK": rope, "zmask": np.zeros((128, 16), np.float32), "icntc": _icnt(0, C, C), "xhc": np.zeros((128, D), np.float32),
        "w_uq": f32(inputs["w_uq"])[0], "w_ukv": f32(inputs["w_ukv"])[0], "odd_w_out": f32(inputs["odd_w_out"])[0],
        "router_wT": np.ascontiguousarray(f32(inputs["router_w"])[0].T),
        "moe_w_gate": f32(inputs["moe_w_gate"])[0], "moe_w_up": f32(inputs["moe_w_up"])[0], "moe_w_down": f32(inputs["moe_w_down"])[0],
        "final_norm_g": f32(inputs["final_norm_g"]),
    }
    p = _get_prog(L, NQ, C, 'AB')
    maps = []
    for core in range(ncores):
        b, qc = core // nqc, core % nqc
        s0 = qc * NQ
        cc = np.zeros((128, 8, 2), np.float32)
        cc[:, :, 0] = c[b].reshape(8, 128).T
        cc[:, :, 1] = c_ctx.reshape(8, 128).T
        xh = np.zeros((128, D), np.float32)
        hm = np.zeros((128, 16), np.float32)
        if s0 >= 8:
            xh[0:8] = x[b, s0 - 8:s0]
            hm[:, 0:8] = 1.0
        if s0 + NQ + 8 <= L:
            xh[8:16] = x[b, s0 + NQ:s0 + NQ + 8]
            hm[:, 8:16] = 1.0
        m = dict(common)
        m.update({"cc": cc, "xb": x[b], "ctxb": ctx[b], "xo": np.ascontiguousarray(x[b, s0:s0 + NQ]), "xh": xh, "hmask": hm,
                  "icnt": _icnt(s0, NQ, L), "ropeQ": np.ascontiguousarray(rope[:, :, s0:s0 + NQ])})
        maps.append({k: m[k] for k in p.inputs})
    res = run_bass_kernel_spmd(p.nc, maps, core_ids=list(range(ncores)))
    out = np.zeros((B, L, D), np.float32)
    for core in range(ncores):
        b, qc = core // nqc, core % nqc
        out[b, qc * NQ:(qc + 1) * NQ] = np.asarray(res.results[core]["out"])
    return out


def _run(L, NQ, C, inputs, only_A=False):
    import ml_dtypes
    B = inputs["x"].shape[0]
    nqc = L // NQ
    ncores = B * nqc
    f32 = lambda a: np.ascontiguousarray(np.asarray(a, dtype=np.float32))
    x, c, ctx, c_ctx = f32(inputs["x"]), f32(inputs["c"]), f32(inputs["ctx"]), f32(inputs["c_ctx"])
    rope = _rope_tables(L)
    ident = np.eye(128, dtype=np.float32)
    rmat = _rmat()
    common = {
        "c_ident": ident, "c_rmat": rmat,
        "mod_w": f32(inputs["mod_w"]), "mod_b": f32(inputs["mod_b"]),
        "norm1_g": f32(inputs["norm1_g"]), "norm2_g": f32(inputs["norm2_g"]),
    }
    lam = np.stack([f32(inputs["lambda_q1"])[0], f32(inputs["lambda_k1"])[0], f32(inputs["lambda_q2"])[0], f32(inputs["lambda_k2"])[0]], axis=0)
    wA = {
        "even_w_in": f32(inputs["even_w_in"])[0], "pool_w": f32(inputs["pool_w"])[0], "pool_scale": f32(inputs["pool_scale"])[0],
        "lam": np.ascontiguousarray(lam), "even_w_out": f32(inputs["even_w_out"])[0],
        "ffn_w_gate": f32(inputs["ffn_w_gate"])[0], "ffn_w_up": f32(inputs["ffn_w_up"])[0], "ffn_w_down": f32(inputs["ffn_w_down"])[0],
        "odd_w_in": f32(inputs["odd_w_in"])[0], "q_norm_g": f32(inputs["q_norm_g"])[0], "kv_norm_g": f32(inputs["kv_norm_g"])[0],
        "ropeK": rope, "zmask": np.zeros((128, 16), np.float32), "icntc": _icnt(0, C, C), "xhc": np.zeros((128, D), np.float32),
    }
    wB = {
        "w_uq": f32(inputs["w_uq"])[0], "w_ukv": f32(inputs["w_ukv"])[0], "odd_w_out": f32(inputs["odd_w_out"])[0],
        "router_wT": np.ascontiguousarray(f32(inputs["router_w"])[0].T),
        "moe_w_gate": f32(inputs["moe_w_gate"])[0], "moe_w_up": f32(inputs["moe_w_up"])[0], "moe_w_down": f32(inputs["moe_w_down"])[0],
        "final_norm_g": f32(inputs["final_norm_g"]),
    }
    pA = _get_prog(L, NQ, C, 'A')
    mapsA = []
    for core in range(ncores):
        b, qc = core // nqc, core % nqc
        s0 = qc * NQ
        cc = np.zeros((128, 8, 2), np.float32)
        cc[:, :, 0] = c[b].reshape(8, 128).T
        cc[:, :, 1] = c_ctx.reshape(8, 128).T
        xh = np.zeros((128, D), np.float32)
        hm = np.zeros((128, 16), np.float32)
        if s0 >= 8:
            xh[0:8] = x[b, s0 - 8:s0]
            hm[:, 0:8] = 1.0
        if s0 + NQ + 8 <= L:
            xh[8:16] = x[b, s0 + NQ:s0 + NQ + 8]
            hm[:, 8:16] = 1.0
        m = dict(common)
        m.update(wA)
        m.update({"cc": cc, "xb": x[b], "ctxb": ctx[b], "xo": np.ascontiguousarray(x[b, s0:s0 + NQ]), "xh": xh, "hmask": hm,
                  "icnt": _icnt(s0, NQ, L), "ropeQ": np.ascontiguousarray(rope[:, :, s0:s0 + NQ])})
        mapsA.append({k: m[k] for k in pA.inputs})
    resA = run_bass_kernel_spmd(pA.nc, mapsA, core_ids=list(range(ncores)))
    rA = resA.results
    if only_A:
        return None, rA, None
    pB = _get_prog(L, NQ, C, 'B')
    mapsB = []
    for core in range(ncores):
        b, qc = core // nqc, core % nqc
        s0 = qc * NQ
        cc = np.zeros((128, 8, 2), np.float32)
        cc[:, :, 0] = c[b].reshape(8, 128).T
        cc[:, :, 1] = c_ctx.reshape(8, 128).T
        latall = np.concatenate([np.asarray(rA[b * nqc + j]["lat"]) for j in range(nqc)] + [np.asarray(rA[b * nqc]["latc"])], axis=2)
        m = dict(common)
        m.update(wB)
        m.update({"cc": cc, "h2": np.asarray(rA[core]["h2"]), "cq": np.asarray(rA[core]["cq"]), "latall": np.ascontiguousarray(latall),
                  "ropeQ": np.ascontiguousarray(rope[:, :, s0:s0 + NQ])})
        mapsB.append({k: m[k] for k in pB.inputs})
    resB = run_bass_kernel_spmd(pB.nc, mapsB, core_ids=list(range(ncores)))
    out = np.zeros((B, L, D), np.float32)
    for core in range(ncores):
        b, qc = core // nqc, core % nqc
        out[b, qc * NQ:(qc + 1) * NQ] = np.asarray(resB.results[core]["out"])
    return out, rA, resB.results


def kernel(**inputs):
    L = inputs["x"].shape[1]
    C = inputs["ctx"].shape[1]
    return _run_fused(L, L // 4, C, inputs)
```

```python
import contextlib
import math
import numpy as np
import concourse.bass as bass
import concourse.mybir as mybir
from concourse.bass_utils import run_bass_kernel_spmd

F32 = mybir.dt.float32
BF16 = mybir.dt.bfloat16
U8 = mybir.dt.uint8
AF = mybir.ActivationFunctionType
ALU = mybir.AluOpType
AX = mybir.AxisListType

D = 1024
DC = 8
FF = 2816
FC = 22
EPS = 1e-6
NEXP = 8
GRID_W = 64
POOL_WINDOWS = (2, 4, 8, 16)
ISZ = {F32: 4, BF16: 2, U8: 1}


class Buf:
    def __init__(self, name, ap=None, multi=False):
        self.name = name
        self.ap = ap
        self.multi = multi
        self.writes = []
        self.reads = {}
        self.war = []
        self.sem = None
        self.sem_count = 0


class Eng:
    def __init__(self, name, sem):
        self.name = name
        self.sem = sem
        self.count = 0
        self.known = {}
        self.items = []


class Sched:
    ENGS = ['tensor', 'vector', 'scalar', 'gpsimd', 'sync']

    def __init__(self, nc, arena_bytes=0):
        self.nc = nc
        self.stack = contextlib.ExitStack()
        self.sems = []
        self.eng = {}
        for n in self.ENGS:
            self.eng[n] = Eng(n, self.new_sem('e_' + n))
        self.dram_bufs = {}
        self.free_sems = []
        self.live = []
        self.all_dma_bufs = []
        self.top = 0
        self.arena = None
        self.arena_bytes = arena_bytes
        if arena_bytes:
            self.arena = self.stack.enter_context(nc.sbuf_tensor("arena", [128, arena_bytes], U8))

    def new_sem(self, name):
        s = self.stack.enter_context(self.nc.semaphore(name))
        self.sems.append(s)
        return len(self.sems) - 1

    def sbuf(self, name, shape, dtype, multi=False):
        t = self.stack.enter_context(self.nc.sbuf_tensor(name, list(shape), dtype))
        return Buf(name, t, multi)

    def psum(self, name, shape, dtype):
        t = self.stack.enter_context(self.nc.psum_tensor(name, list(shape), dtype))
        return Buf(name, t)

    def alloc(self, name, shape, dtype, multi=False):
        n = 1
        for s in shape:
            n *= s
        nb = n * ISZ[dtype]
        nb_al = (nb + 63) // 64 * 64
        off = self.top
        assert off + nb_al <= self.arena_bytes, (name, off, nb_al, self.arena_bytes)
        self.top += nb_al
        ap = self.arena[:, off:off + nb]
        if dtype != U8:
            ap = ap.bitcast(dtype)
        if len(shape) == 2:
            ap = ap.rearrange("p (a b) -> p a b", a=shape[0])
        elif len(shape) == 3:
            ap = ap.rearrange("p (a b c) -> p a b c", a=shape[0], b=shape[1])
        b = Buf(name, ap, multi)
        self.live.append((off, b))
        return b

    def view(self, name, ap, multi=False):
        b = Buf(name, ap, multi)
        self.live.append((self.top, b))
        return b

    def mark(self):
        return self.top

    def release(self, mark):
        keep = []
        for off, b in self.live:
            if off >= mark:
                if b.sem is not None:
                    self.free_sems.append((b.sem, b.sem_count))
                    b.sem = None
            else:
                keep.append((off, b))
        self.live = keep
        self.top = mark

    def dram_buf(self, name, multi=True):
        if name not in self.dram_bufs:
            self.dram_bufs[name] = Buf('dram_' + name, None, multi)
        return self.dram_bufs[name]

    def _deps(self, reads, writes):
        deps = []
        for b in reads:
            deps.extend(b.writes)
        for b in writes:
            if b.reads:
                b.war = list(b.reads.values())
                b.reads = {}
                b.writes = []
            deps.extend(b.war)
            if not b.multi:
                deps.extend(b.writes)
        return deps

    def _commit(self, tok, reads, writes):
        for b in reads:
            b.reads[tok[0]] = tok
        for b in writes:
            if b.multi:
                b.writes = [t for t in b.writes if t[0] != tok[0]] + [tok]
            else:
                b.writes = [tok]

    def _waits(self, E, deps):
        best = {}
        for (s, v, clk) in deps:
            if E.name == 'tensor' and s == E.sem:
                continue
            if E.known.get(s, 0) >= v:
                continue
            best[s] = max(best.get(s, 0), v)
            E.known[s] = v
            for k, kv in clk.items():
                if E.known.get(k, 0) < kv:
                    E.known[k] = kv
        return list(best.items())

    def op(self, eng, reads, writes, emit):
        E = self.eng[eng]
        ws = self._waits(E, self._deps(reads, writes))
        E.count += 1
        tok = (E.sem, E.count, dict(E.known))
        E.items.append((ws, emit, (E.sem, 1)))
        self._commit(tok, reads, writes)
        return tok

    def dma(self, queue, out_ap, in_ap, reads, writes, **kw):
        E = self.eng[queue]
        dst = writes[0]
        if dst.sem is None:
            if self.free_sems:
                dst.sem, dst.sem_count = self.free_sems.pop()
            else:
                dst.sem = self.new_sem('d%d' % len(self.sems))
            self.all_dma_bufs.append(dst)
        ws = self._waits(E, self._deps(reads, writes))
        dst.sem_count += 16
        tok = (dst.sem, dst.sem_count, dict(E.known))
        E.items.append((ws, (lambda e: e.dma_start(out=out_ap, in_=in_ap, **kw)), (dst.sem, 16)))
        self._commit(tok, reads, writes)
        return tok

    def dma_custom(self, queue, emit, reads, writes, inc=16):
        E = self.eng[queue]
        dst = writes[0]
        if dst.sem is None:
            if self.free_sems:
                dst.sem, dst.sem_count = self.free_sems.pop()
            else:
                dst.sem = self.new_sem('d%d' % len(self.sems))
            self.all_dma_bufs.append(dst)
        ws = self._waits(E, self._deps(reads, writes))
        dst.sem_count += inc
        tok = (dst.sem, dst.sem_count, dict(E.known))
        E.items.append((ws, emit, (dst.sem, inc)))
        self._commit(tok, reads, writes)
        return tok

    def barrier(self):
        deps = []
        for n in self.ENGS:
            E = self.eng[n]
            if E.count:
                deps.append((E.sem, E.count, {}))
        seen = set()
        for b in self.all_dma_bufs:
            if b.sem is not None and b.sem not in seen:
                seen.add(b.sem)
                deps.append((b.sem, b.sem_count, {}))
        for (s, c) in self.free_sems:
            if s not in seen:
                seen.add(s)
                deps.append((s, c, {}))
        for n in self.ENGS:
            E = self.eng[n]
            ws = []
            for (s, v, _) in deps:
                if E.known.get(s, 0) < v and not (n == 'tensor' and s == E.sem):
                    ws.append((s, v))
                    E.known[s] = v
            if ws:
                E.items.append((ws, None, None))

    def rotate(self, limit=20000):
        self.barrier()
        for n in self.ENGS:
            E = self.eng[n]
            if E.count > limit:
                E.sem = self.new_sem('e2_%s_%d' % (n, len(self.sems)))
                E.count = 0

    def finish(self):
        self.barrier()
        sems = self.sems
        with self.nc.Block() as block:
            for n in self.ENGS:
                items = self.eng[n].items

                def body(e, items=items):
                    for ws, emit, inc in items:
                        for s, v in ws:
                            e.wait_ge(sems[s], v)
                        if emit is not None:
                            ins = emit(e)
                            ins.then_inc(sems[inc[0]], inc[1])
                getattr(block, n)(body)
        self.stack.close()
        return sum(len(self.eng[n].items) for n in self.ENGS)


class Prog:
    def __init__(self, L, NQ, C, stage):
        self.L, self.NQ, self.C, self.stage = L, NQ, C, stage
        self.NK = L + C
        self.NT = self.NK // 128
        nc = bass.Bass("TRN2", target_bir_lowering=False)
        self.nc = nc
        self.S = Sched(nc, arena_bytes=200 * 1024)
        self.inputs = {}
        self.outputs = {}
        S = self.S
        self.PS = [S.psum("ps%d" % i, [128, 512], F32) for i in range(8)]
        self.tr_ctr = 0

    def inp(self, name, shape, dtype=F32):
        t = self.nc.dram_tensor(name, list(shape), dtype, kind="ExternalInput")
        self.inputs[name] = (tuple(shape), dtype)
        return t.ap()

    def outp(self, name, shape, dtype=F32):
        t = self.nc.dram_tensor(name, list(shape), dtype, kind="ExternalOutput")
        self.outputs[name] = (tuple(shape), dtype)
        return t.ap()

    def scratch(self, name, shape, dtype):
        return self.nc.dram_tensor(name, list(shape), dtype).ap()

    def mm(self, ps, out_ap, lhs, rhs, reads, start=True, stop=True):
        def emit(e):
            n = len(lhs)
            for i in range(n):
                ins = e.matmul(out_ap, lhsT=lhs[i], rhs=rhs[i], start=(start and i == 0), stop=(stop and i == n - 1))
            return ins
        return self.S.op('tensor', reads, [ps], emit)

    def act(self, out_b, out_ap, in_b, in_ap, func, reads=(), **kw):
        return self.S.op('scalar', [in_b] + list(reads), [out_b],
                         lambda e: e.activation(out=out_ap, in_=in_ap, func=func, **kw))

    def tt(self, eng, out_b, out_ap, a_b, a_ap, b_b, b_ap, op):
        return self.S.op(eng, [a_b, b_b], [out_b],
                         lambda e: e.tensor_tensor(out=out_ap, in0=a_ap, in1=b_ap, op=op))

    def stt(self, out_b, out_ap, a_b, a_ap, scalar, b_b, b_ap, op0, op1, reads=(), writes=(), **kw):
        return self.S.op('vector', [a_b, b_b] + list(reads), [out_b] + list(writes),
                         lambda e: e.scalar_tensor_tensor(out=out_ap, in0=a_ap, scalar=scalar, in1=b_ap, op0=op0, op1=op1, **kw))

    def ts(self, eng, out_b, out_ap, a_b, a_ap, s1, s2, op0, op1=None, reads=()):
        if op1 is None:
            return self.S.op(eng, [a_b] + list(reads), [out_b],
                             lambda e: e.tensor_scalar(out=out_ap, in0=a_ap, scalar1=s1, scalar2=None, op0=op0))
        return self.S.op(eng, [a_b] + list(reads), [out_b],
                         lambda e: e.tensor_scalar(out=out_ap, in0=a_ap, scalar1=s1, scalar2=s2, op0=op0, op1=op1))

    def cp(self, eng, out_b, out_ap, in_b, in_ap):
        if eng == 'scalar':
            return self.S.op(eng, [in_b], [out_b], lambda e: e.copy(out=out_ap, in_=in_ap))
        return self.S.op(eng, [in_b], [out_b], lambda e: e.tensor_copy(out=out_ap, in_=in_ap))

    def memset(self, eng, b, ap, val):
        return self.S.op(eng, [], [b], lambda e: e.memset(ap, val))

    def load(self, b, out_ap, in_ap, src=None, q='sync', **kw):
        return self.S.dma(q, out_ap, in_ap, [src] if src is not None else [], [b], **kw)

    def store(self, dst, out_ap, b, in_ap, q='sync', **kw):
        return self.S.dma(q, out_ap, in_ap, [b], [dst], **kw)

    def setup_consts(self):
        S = self.S
        self.ident = S.alloc("ident", [128], BF16)
        self.rmat = S.alloc("rmat", [128], BF16)
        self.onesb = S.alloc("onesb", [128], BF16)
        self.epsc = S.alloc("epsc", [1], F32)
        c_ident = self.inp("c_ident", [128, 128])
        c_rmat = self.inp("c_rmat", [128, 128])
        self.load(self.ident, self.ident.ap[:], c_ident, q='gpsimd')
        self.load(self.rmat, self.rmat.ap[:], c_rmat, q='gpsimd')
        self.memset('vector', self.onesb, self.onesb.ap[:], 1.0)
        self.memset('vector', self.epsc, self.epsc.ap[:], EPS)

    def mod_prologue(self, layers):
        S = self.S
        m0 = S.mark()
        cc = self.inp("cc", [128, 8, 2])
        mod_w = self.inp("mod_w", [2, D, 6 * D])
        mod_b = self.inp("mod_b", [2, 6 * D])
        n1g = self.inp("norm1_g", [2, D])
        n2g = self.inp("norm2_g", [2, D])
        self.MODB = self.scratch("MODB", [2, 2, 6, D], F32)
        self.modb_buf = S.dram_buf("MODB")
        ccs = S.alloc("ccs", [8, 2], F32)
        sct = S.alloc("sct", [8, 2], F32)
        self.load(ccs, ccs.ap[:], cc)
        self.act(sct, sct.ap[:], ccs, ccs.ap[:], AF.Silu)
        mw = [S.alloc("mw%d" % i, [8, 512], F32) for i in range(2)]
        row = S.alloc("modrow", [6 * D], F32)
        brow = S.alloc("modbrow", [6 * D], F32)
        g1r = S.alloc("g1r", [D], F32)
        g2r = S.alloc("g2r", [D], F32)
        orow = S.alloc("orow", [6, D], F32)
        ps = self.PS
        k = 0
        for i in layers:
            self.load(brow, brow.ap[0:2, :], mod_b[i:i + 1, :].partition_broadcast(2))
            self.load(g1r, g1r.ap[0:2, :], n1g[i:i + 1, :].partition_broadcast(2))
            self.load(g2r, g2r.ap[0:2, :], n2g[i:i + 1, :].partition_broadcast(2))
            for n in range(12):
                w = mw[k % 2]
                self.load(w, w.ap[:], mod_w[i, :, n * 512:(n + 1) * 512].rearrange("(c p) n -> p c n", p=128))
                pb = ps[k % 2]
                self.mm(pb, pb.ap[0:2, :], [sct.ap[:, c, :] for c in range(8)], [w.ap[:, c, :] for c in range(8)], [sct, w])
                self.tt('vector', row, row.ap[0:2, n * 512:(n + 1) * 512], pb, pb.ap[0:2, :], brow, brow.ap[0:2, n * 512:(n + 1) * 512], ALU.add)
                k += 1
            r = lambda j: row.ap[0:2, j * D:(j + 1) * D]
            self.stt(orow, orow.ap[0:2, 0, :], row, r(1), 1.0, g1r, g1r.ap[0:2, :], ALU.add, ALU.mult)
            self.cp('vector', orow, orow.ap[0:2, 1, :], row, r(0))
            self.cp('vector', orow, orow.ap[0:2, 2, :], row, r(2))
            self.stt(orow, orow.ap[0:2, 3, :], row, r(4), 1.0, g2r, g2r.ap[0:2, :], ALU.add, ALU.mult)
            self.cp('vector', orow, orow.ap[0:2, 4, :], row, r(3))
            self.cp('vector', orow, orow.ap[0:2, 5, :], row, r(5))
            self.store(self.modb_buf, self.MODB[i], orow, orow.ap[0:2, :, :])
        S.barrier()
        S.release(m0)

    def load_mod(self, name, layer, which, rowi):
        b = self.S.alloc(name, [D], F32)
        self.load(b, b.ap[:], self.MODB[layer, which, rowi:rowi + 1, :].partition_broadcast(128), src=self.modb_buf)
        return b

    def alloc_norm_tmp(self, tag):
        S = self.S
        t = {}
        t['junk'] = S.alloc(tag + "junk", [D], BF16)
        t['ss'] = [S.alloc(tag + "ss%d" % i, [8], F32, multi=True) for i in range(2)]
        t['tmp'] = [S.alloc(tag + "tmp%d" % i, [D], F32) for i in range(2)]
        t['ab'] = [S.alloc(tag + "ab%d" % i, [D], BF16) for i in range(2)]
        t['af'] = [S.alloc(tag + "af%d" % i, [D], F32) for i in range(2)]
        t['k'] = 0
        return t

    def norm_mod_T(self, t, xt, A, B, aT, want_f32=False, tr_banks=(0, 1)):
        n = len(xt)
        ssb = t['ss'][t['k'] % 2]
        afs = []
        for i, (xb, xap) in enumerate(xt):
            self.S.op('scalar', [xb], [t['junk'], ssb],
                      lambda e, xap=xap, i=i: e.activation(out=t['junk'].ap[:], in_=xap, func=AF.Square, accum_out=ssb.ap[:, i:i + 1]))
        self.act(ssb, ssb.ap[:, 4:4 + n], ssb, ssb.ap[:, 0:n], AF.Sqrt, reads=[self.epsc], scale=1.0 / D, bias=self.epsc.ap[:])
        self.S.op('vector', [ssb], [ssb], lambda e: e.reciprocal(out=ssb.ap[:, 4:4 + n], in_=ssb.ap[:, 4:4 + n]))
        for i, (xb, xap) in enumerate(xt):
            k = t['k']
            t['k'] += 1
            tmp = t['tmp'][k % 2]
            ab = t['ab'][k % 2]
            self.stt(tmp, tmp.ap[:], xb, xap, ssb.ap[:, 4 + i:5 + i], A, A.ap[:], ALU.mult, ALU.mult, reads=[ssb])
            if want_f32:
                af = t['af'][k % 2]
                self.tt('gpsimd', af, af.ap[:], tmp, tmp.ap[:], B, B.ap[:], ALU.add)
                self.cp('scalar', ab, ab.ap[:], af, af.ap[:])
                afs.append(af)
            else:
                self.tt('gpsimd', ab, ab.ap[:], tmp, tmp.ap[:], B, B.ap[:], ALU.add)
            pb = self.PS[tr_banks[self.tr_ctr % 2]]
            self.tr_ctr += 1
            pv = pb.ap[:].bitcast(BF16).rearrange("p (c n) -> p c n", c=8)

            def tr(e, ab=ab, pv=pv):
                for c in range(8):
                    ins = e.transpose(out=pv[:, c, :], in_=ab.ap[:, c * 128:(c + 1) * 128], identity=self.ident.ap[:])
                return ins
            self.S.op('tensor', [ab, self.ident], [pb], tr)
            ob, oap = aT[i]
            self.cp('scalar', ob, oap, pb, pv)
            if want_f32:
                yield af

    def run_norm(self, *a, **kw):
        return list(self.norm_mod_T(*a, **kw))

    def rope_tile(self, rows, G, zp, kz, rp, t1, t2, cosb, cos_ap, sinb, sin_ap, ob, oap):
        self.cp('scalar', kz, kz.ap[0:rows, 0:G], zp, zp.ap[0:rows, 0:G])
        self.mm(rp, rp.ap[0:rows, 0:G], [self.rmat.ap[0:rows, 0:rows]], [kz.ap[0:rows, 0:G]], [self.rmat, kz])
        self.tt('vector', t1, t1.ap[0:rows, 0:G], kz, kz.ap[0:rows, 0:G], cosb, cos_ap, ALU.mult)
        self.tt('vector', t2, t2.ap[0:rows, 0:G], rp, rp.ap[0:rows, 0:G], sinb, sin_ap, ALU.mult)
        self.tt('gpsimd', ob, oap, t1, t1.ap[0:rows, 0:G], t2, t2.ap[0:rows, 0:G], ALU.add)

    def l0_kv_build(self):
        S, L, C = self.S, self.L, self.C
        m0 = S.mark()
        xb = self.inp("xb", [L, D])
        ctxb = self.inp("ctxb", [C, D])
        ropeK = self.inp("ropeK", [2, 128, L])
        self.w_in0 = self.inp("even_w_in", [D, 2560])
        self.KT0 = self.scratch("KT0", [6, 128, self.NK], BF16)
        self.V0 = self.scratch("V0", [6, 128, self.NT, 128], BF16)
        self.kt0_buf = S.dram_buf("KT0")
        self.v0_buf = S.dram_buf("V0")
        Wk = S.alloc("Wk", [8, 768], BF16)
        Wv = S.alloc("Wv", [8, 768], BF16)
        self.load(Wk, Wk.ap[:], self.w_in0[:, 1024:1792].rearrange("(c p) n -> p c n", p=128), q='gpsimd')
        self.load(Wv, Wv.ap[:], self.w_in0[:, 1792:2560].rearrange("(c p) n -> p c n", p=128), q='gpsimd')
        A = [self.load_mod("kvA%d" % w, 0, w, 0) for w in range(2)]
        B = [self.load_mod("kvB%d" % w, 0, w, 1) for w in range(2)]
        t = self.alloc_norm_tmp("kv")
        xg = [S.alloc("kvx%d" % i, [4, D], F32) for i in range(2)]
        aT = [S.alloc("kvaT%d" % i, [8, 512], BF16) for i in range(2)]
        cs = [S.alloc("kvcs%d" % i, [2, 512], F32) for i in range(2)]
        kz = [S.alloc("kvkz%d" % i, [512], BF16) for i in range(2)]
        t1 = [S.alloc("kvt1%d" % i, [512], F32) for i in range(2)]
        t2 = [S.alloc("kvt2%d" % i, [512], F32) for i in range(2)]
        kst = [S.alloc("kvkst%d" % i, [512], BF16) for i in range(4)]
        vst = [S.alloc("kvvst%d" % i, [6, 4, 128], BF16) for i in range(2)]
        PS = self.PS
        groups = []
        for g in range(L // 512):
            groups.append((0, g * 512, 512))
        for g in range((C + 511) // 512):
            groups.append((1, g * 512, min(512, C - g * 512)))

        def issue_load(gi):
            which, t0, G = groups[gi]
            nt = G // 128
            src = xb if which == 0 else ctxb
            xs = xg[gi % 2]
            self.load(xs, xs.ap[:, 0:nt, :], src[t0:t0 + G, :].rearrange("(t p) d -> p t d", p=128))
            if which == 0:
                c_ = cs[gi % 2]
                self.load(c_, c_.ap[:, :, 0:G], ropeK[:, :, t0:t0 + G].rearrange("a p n -> p a n"))
        issue_load(0)
        kk = 0
        for gi, (which, t0, G) in enumerate(groups):
            if gi + 1 < len(groups):
                issue_load(gi + 1)
            nt = G // 128
            xs = xg[gi % 2]
            aTs = aT[gi % 2]
            c_ = cs[gi % 2]
            self.run_norm(t, [(xs, xs.ap[:, i, :]) for i in range(nt)], A[which], B[which],
                          [(aTs, aTs.ap[:, :, i * 128:(i + 1) * 128]) for i in range(nt)])
            key0 = t0 if which == 0 else L + t0
            for h in range(6):
                zp = PS[2 + kk % 2]
                rp = PS[4 + kk % 2]
                ks = kst[kk % 4]
                self.mm(zp, zp.ap[:, 0:G], [Wk.ap[:, c, h * 128:(h + 1) * 128] for c in range(8)],
                        [aTs.ap[:, c, 0:G] for c in range(8)], [Wk, aTs])
                if which == 0:
                    self.rope_tile(128, G, zp, kz[kk % 2], rp, t1[kk % 2], t2[kk % 2],
                                   c_, c_.ap[:, 0, 0:G], c_, c_.ap[:, 1, 0:G], ks, ks.ap[:, 0:G])
                else:
                    self.cp('scalar', ks, ks.ap[:, 0:G], zp, zp.ap[:, 0:G])
                self.store(self.kt0_buf, self.KT0[h, :, key0:key0 + G], ks, ks.ap[:, 0:G])
                kk += 1
            vs = vst[gi % 2]
            for i in range(nt):
                va, vb = PS[6], PS[7]
                lhs = [aTs.ap[:, c, i * 128:(i + 1) * 128] for c in range(8)]
                self.mm(va, va.ap[:, 0:512], lhs, [Wv.ap[:, c, 0:512] for c in range(8)], [Wv, aTs])
                self.mm(vb, vb.ap[:, 0:256], lhs, [Wv.ap[:, c, 512:768] for c in range(8)], [Wv, aTs])
                self.cp('vector', vs, vs.ap[:, 0:4, i, :], va, va.ap[:, 0:512].rearrange("p (h d) -> p h d", h=4))
                self.cp('vector', vs, vs.ap[:, 4:6, i, :], vb, vb.ap[:, 0:256].rearrange("p (h d) -> p h d", h=2))
            tile0 = key0 // 128
            self.store(self.v0_buf, self.V0[:, :, tile0:tile0 + nt, :].rearrange("h p t d -> p h t d"), vs, vs.ap[:, :, 0:nt, :])
        S.barrier()
        S.release(m0)

    def l0_q_build(self, tag, xq, nq, xh, hmask, which, ropeQ, QTs, qt_buf, pext):
        S = self.S
        m0 = S.mark()
        qst = [S.alloc("qst%d" % i, [512], BF16) for i in range(4)]
        Wq = S.alloc("Wq", [8, 768], BF16)
        Wp = S.alloc("Wp", [8, 256], BF16)
        self.load(Wq, Wq.ap[:], self.w_in0[:, 256:1024].rearrange("(c p) n -> p c n", p=128), q='gpsimd')
        self.load(Wp, Wp.ap[:], self.w_in0[:, 0:256].rearrange("(c p) n -> p c n", p=128), q='gpsimd')
        A = self.load_mod("qA", 0, which, 0)
        B = self.load_mod("qB", 0, which, 1)
        t = self.alloc_norm_tmp("q")
        xg = [S.alloc("qx%d" % i, [4, D], F32) for i in range(2)]
        aT = [S.alloc("qaT%d" % i, [8, 512], BF16) for i in range(2)]
        cs = [S.alloc("qcs%d" % i, [2, 512], F32) for i in range(2)]
        kz = [S.alloc("qkz%d" % i, [512], BF16) for i in range(2)]
        t1 = [S.alloc("qt1%d" % i, [512], F32) for i in range(2)]
        t2 = [S.alloc("qt2%d" % i, [512], F32) for i in range(2)]
        hm = S.alloc("qhm", [16], F32)
        self.load(hm, hm.ap[:], hmask)
        PS = self.PS
        ngr = (nq + 511) // 512
        groups = [(g * 512, min(512, nq - g * 512)) for g in range(ngr)] + [(-1, 128)]

        def issue_load(gi):
            t0, G = groups[gi]
            xs = xg[gi % 2]
            if t0 < 0:
                self.load(xs, xs.ap[:, 0:1, :], xh.rearrange("(t p) d -> p t d", p=128))
                return
            self.load(xs, xs.ap[:, 0:G // 128, :], xq[t0:t0 + G, :].rearrange("(t p) d -> p t d", p=128))
            if ropeQ is not None:
                c_ = cs[gi % 2]
                self.load(c_, c_.ap[:, :, 0:G], ropeQ[:, :, t0:t0 + G].rearrange("a p n -> p a n"))
        issue_load(0)
        kk = 0
        for gi, (t0, G) in enumerate(groups):
            if gi + 1 < len(groups):
                issue_load(gi + 1)
            nt = G // 128
            xs = xg[gi % 2]
            aTs = aT[gi % 2]
            c_ = cs[gi % 2]
            self.run_norm(t, [(xs, xs.ap[:, i, :]) for i in range(nt)], A, B,
                          [(aTs, aTs.ap[:, :, i * 128:(i + 1) * 128]) for i in range(nt)])
            if t0 < 0:
                for j in range(2):
                    zp = PS[2 + j]
                    self.mm(zp, zp.ap[:, 0:16], [Wp.ap[:, c, j * 128:(j + 1) * 128] for c in range(8)],
                            [aTs.ap[:, c, 0:16] for c in range(8)], [Wp, aTs])
                    self.tt('vector', pext, pext.ap[:, j, 0:8], zp, zp.ap[:, 0:8], hm, hm.ap[:, 0:8], ALU.mult)
                    self.tt('vector', pext, pext.ap[:, j, 8 + nq:16 + nq], zp, zp.ap[:, 8:16], hm, hm.ap[:, 8:16], ALU.mult)
                continue
            for j in range(2):
                zp = PS[2 + kk % 2]
                self.mm(zp, zp.ap[:, 0:G], [Wp.ap[:, c, j * 128:(j + 1) * 128] for c in range(8)],
                        [aTs.ap[:, c, 0:G] for c in range(8)], [Wp, aTs])
                self.cp('scalar', pext, pext.ap[:, j, 8 + t0:8 + t0 + G], zp, zp.ap[:, 0:G])
                kk += 1
            for h in range(6):
                zp = PS[2 + kk % 2]
                rp = PS[4 + kk % 2]
                self.mm(zp, zp.ap[:, 0:G], [Wq.ap[:, c, h * 128:(h + 1) * 128] for c in range(8)],
                        [aTs.ap[:, c, 0:G] for c in range(8)], [Wq, aTs])
                qb = qst[kk % 4]
                if ropeQ is not None:
                    self.rope_tile(128, G, zp, kz[kk % 2], rp, t1[kk % 2], t2[kk % 2],
                                   c_, c_.ap[:, 0, 0:G], c_, c_.ap[:, 1, 0:G], qb, qb.ap[:, 0:G])
                else:
                    self.cp('scalar', qb, qb.ap[:, 0:G], zp, zp.ap[:, 0:G])
                self.store(qt_buf, QTs[h, :, t0:t0 + G], qb, qb.ap[:, 0:G])
                kk += 1
        S.barrier()
        S.release(m0)

    def l0_pool(self, nq, pext, icnt_dram, OT, ot_buf):
        S = self.S
        m0 = S.mark()
        n = nq
        sA = S.alloc("plA", [2, n + 16], F32)
        sB = S.alloc("plB", [2, n + 16], F32)
        ic = S.alloc("plic", [2, n], F32)
        pd = S.alloc("plpd", [2, n], BF16)
        pw = S.alloc("plw", [2, 128], BF16)
        psc = S.alloc("plsc", [2], F32)
        pool_w = self.inp_once("pool_w", [4, 64, 64])
        pool_scale = self.inp_once("pool_scale", [256])
        self.load(ic, ic.ap[:], icnt_dram)
        self.memset('vector', pw, pw.ap[:], 0.0)
        for g in range(4):
            j, hf = g // 2, g % 2
            self.load(pw, pw.ap[hf * 64:(hf + 1) * 64, j, hf * 64:(hf + 1) * 64], pool_w[g], q='gpsimd')
        self.load(psc, psc.ap[:], pool_scale.rearrange("(j p) -> p j", p=128), allow_slow_non_contiguous=True)
        eng = 'gpsimd'
        for g, w in enumerate(POOL_WINDOWS):
            j, hf = g // 2, g % 2
            r = slice(hf * 64, (hf + 1) * 64)
            u = pext.ap[r, j, :]
            a = sA.ap[r, j, :]
            b = sB.ap[r, j, :]
            N = n + 16
            self.tt(eng, sA, a[:, 1:N], pext, u[:, 0:N - 1], pext, u[:, 1:N], ALU.add)
            fin, finb = a, sA
            if w >= 4:
                self.tt(eng, sB, b[:, 2:N - 1], sA, a[:, 1:N - 2], sA, a[:, 3:N], ALU.add)
                fin, finb = b, sB
            if w >= 8:
                self.tt(eng, sA, a[:, 4:N - 3], sB, b[:, 2:N - 5], sB, b[:, 6:N - 1], ALU.add)
                fin, finb = a, sA
            if w >= 16:
                self.tt(eng, sB, b[:, 8:N - 7], sA, a[:, 4:N - 11], sA, a[:, 12:N - 3], ALU.add)
                fin, finb = b, sB
            other = sB if finb is sA else sA
            oth = other.ap[r, j, :]
            self.tt('vector', other, oth[:, 8:8 + n], finb, fin[:, 8:8 + n], ic, ic.ap[r, j, :], ALU.mult)
            self.tt('vector', pd, pd.ap[r, j, :], other, oth[:, 8:8 + n], pext, u[:, 8:8 + n], ALU.subtract)
        stg = [S.alloc("plst%d" % i, [512], BF16) for i in range(2)]
        kk = 0
        for j in range(2):
            for t0 in range(0, n, 512):
                G = min(512, n - t0)
                zp = self.PS[kk % 2]
                st = stg[kk % 2]
                self.mm(zp, zp.ap[:, 0:G], [pw.ap[:, j, :]], [pd.ap[:, j, t0:t0 + G]], [pw, pd])
                self.act(st, st.ap[:, 0:G], zp, zp.ap[:, 0:G], AF.Copy, reads=[psc], scale=psc.ap[:, j:j + 1])
                self.store(ot_buf, OT[j, :, t0:t0 + G], st, st.ap[:, 0:G])
                kk += 1
        S.barrier()
        S.release(m0)

    def inp_once(self, name, shape, dtype=F32):
        if not hasattr(self, '_inp_cache'):
            self._inp_cache = {}
        if name not in self._inp_cache:
            self._inp_cache[name] = self.inp(name, shape, dtype)
        return self._inp_cache[name]

    def attention(self, cfg):
        S = self.S
        nh, nq, key0, nkeys, maps = cfg['nh'], cfg['nq'], cfg['key0'], cfg['nkeys'], cfg['maps']
        ntl = nkeys // 128
        nt0 = (ntl + 1) // 2
        halves = [(0, nt0), (nt0, ntl)] if ntl > 1 else [(0, 1)]
        m0 = S.mark()
        Kb = [S.alloc("atK%d" % i, [(b - a) * 128], BF16, multi=True) for i, (a, b) in enumerate(halves)]
        Vb = [S.alloc("atV%d" % i, [(b - a), 128], BF16, multi=True) for i, (a, b) in enumerate(halves)]
        NPT = 4
        PT = [[S.alloc("atP%d_%d" % (m, i), [512], BF16) for i in range(NPT)] for m in range(maps)]
        SA = [[S.alloc("atSA%d_%d" % (m, i), [512], BF16) for i in range(2)] for m in range(maps)]
        SB = [[S.alloc("atSB%d_%d" % (m, i), [512], BF16) for i in range(2)] for m in range(maps)]
        SC = [[S.alloc("atSC%d_%d" % (m, i), [512], BF16) for i in range(2)] for m in range(maps)]
        PS = self.PS
        if maps == 2:
            Sb = [[PS[0], PS[1]], [PS[2], PS[3]]]
            Ob = [[PS[4]], [PS[5]]]
            Lb = [[PS[6]], [PS[7]]]
        else:
            Sb = [[PS[0], PS[1]]]
            Ob = [[PS[2], PS[3]]]
            Lb = [[PS[4], PS[5]]]
        cfg['free_banks'] = [PS[6], PS[7]] if maps == 1 else []
        scale = cfg['scale']
        pctr = 0
        cctr = 0
        for h in range(nh):
            KT = cfg['KT'](h)
            V = cfg['V'](h)
            for i, (a, b) in enumerate(halves):
                self.load(Kb[i], Kb[i].ap[:, :], KT[:, key0 + a * 128:key0 + b * 128], src=cfg['kbuf'])
                self.load(Vb[i], Vb[i].ap[:, :, :], V[:, key0 // 128 + a:key0 // 128 + b, :], src=cfg['vbuf'])
            qh = cfg['qprov'](h)
            nchunk = (nq + 511) // 512
            for qc in range(nchunk):
                G = min(512, nq - qc * 512)
                qinfo = qh(qc)
                O = [Ob[m][cctr % len(Ob[m])] for m in range(maps)]
                Lq = [Lb[m][cctr % len(Lb[m])] for m in range(maps)]
                cctr += 1
                pend = None
                hist = [[] for _ in range(maps)]
                pendL = [[] for _ in range(maps)]
                for kt in range(ntl + 1):
                    cur = None
                    if kt < ntl:
                        hi = 0 if kt < halves[0][1] else 1
                        kl = kt - halves[hi][0]
                        cur = []
                        for m in range(maps):
                            sb = Sb[m][kt % 2]
                            pt = PT[m][pctr % NPT]
                            parts = qinfo[m]

                            def emit(e, parts=parts, sb=sb, hi=hi, kl=kl, G=G):
                                n = len(parts)
                                for i, (kind, qb, qap, rows) in enumerate(parts):
                                    if kind == 'main':
                                        lhs = Kb[hi].ap[rows[0]:rows[1], kl * 128:(kl + 1) * 128]
                                    else:
                                        lhs = cfg['KR'].ap[rows[0]:rows[1], key0 + (halves[hi][0] + kl) * 128:key0 + (halves[hi][0] + kl + 1) * 128]
                                    ins = e.matmul(sb.ap[:, 0:G], lhsT=lhs, rhs=qap, start=(i == 0), stop=(i == n - 1))
                                return ins
                            rd = [Kb[hi]] + [p[1] for p in parts] + ([cfg['KR']] if any(p[0] == 'rope' for p in parts) else [])
                            S.op('tensor', rd, [sb], emit)
                            self.act(pt, pt.ap[:, 0:G], sb, sb.ap[:, 0:G], AF.Exp, scale=scale)
                            cur.append((pt, hi, kl))
                        pctr += 1
                    if pend is not None:
                        ktp = kt - 1
                        for m in range(maps):
                            pt, hi, kl = pend[m]
                            self.mm(O[m], O[m].ap[:, 0:G], [Vb[hi].ap[:, kl, :]], [pt.ap[:, 0:G]], [Vb[hi], pt],
                                    start=(ktp == 0), stop=(ktp == ntl - 1))
                            gpos = ktp % 4
                            gidx = ktp // 4
                            gsize = min(4, ntl - gidx * 4)
                            hist[m].append(pt)
                            if gpos == 1:
                                a_ = SA[m][gidx % 2]
                                self.tt('vector', a_, a_.ap[:, 0:G], hist[m][0], hist[m][0].ap[:, 0:G], hist[m][1], hist[m][1].ap[:, 0:G], ALU.add)
                            if gpos == gsize - 1:
                                tl = hist[m]
                                hist[m] = []
                                if gsize == 1:
                                    src = tl[0]
                                elif gsize == 2:
                                    src = SA[m][gidx % 2]
                                elif gsize == 3:
                                    src = SC[m][gidx % 2]
                                    a_ = SA[m][gidx % 2]
                                    self.tt('vector', src, src.ap[:, 0:G], a_, a_.ap[:, 0:G], tl[2], tl[2].ap[:, 0:G], ALU.add)
                                else:
                                    a_ = SA[m][gidx % 2]
                                    b_ = SB[m][gidx % 2]
                                    src = SC[m][gidx % 2]
                                    self.tt('vector', b_, b_.ap[:, 0:G], tl[2], tl[2].ap[:, 0:G], tl[3], tl[3].ap[:, 0:G], ALU.add)
                                    self.tt('vector', src, src.ap[:, 0:G], a_, a_.ap[:, 0:G], b_, b_.ap[:, 0:G], ALU.add)
                                pendL[m].append((src, gidx, kt))
                            ngrp = (ntl + 3) // 4
                            while pendL[m] and (kt - pendL[m][0][2] >= 3 or kt == ntl):
                                src_, gi_, _ = pendL[m].pop(0)
                                self.mm(Lq[m], Lq[m].ap[:, 0:G], [self.onesb.ap[:]], [src_.ap[:, 0:G]], [self.onesb, src_],
                                        start=(gi_ == 0), stop=(gi_ == ngrp - 1))
                    pend = cur
                cfg['epilogue'](h, qc, G, O, Lq)
        S.barrier()
        S.release(m0)

    def l0_attention(self, nq, key0, nkeys, QTs, qt_buf, OT, ot_buf, lam_init):
        S = self.S
        m0 = S.mark()
        lamp = self.inp_once("lam", [4, 64])
        lt = S.alloc("lamt", [256], F32)
        qh_b = [S.alloc("l0q%d" % i, [nq], BF16) for i in range(2)]
        ls = S.alloc("lams", [8], F32)
        lj = S.alloc("lamj", [64], F32)
        self.load(lt, lt.ap[:], lamp.rearrange("a n -> (a n)").rearrange("(o n) -> o n", o=1).partition_broadcast(128))
        self.stt(lj, lj.ap[:], lt, lt.ap[:, 0:64], 1.0, lt, lt.ap[:, 64:128], ALU.mult, ALU.mult, writes=[ls], accum_out=ls.ap[:, 0:1])
        self.stt(lj, lj.ap[:], lt, lt.ap[:, 128:192], 1.0, lt, lt.ap[:, 192:256], ALU.mult, ALU.mult, writes=[ls], accum_out=ls.ap[:, 1:2])
        self.act(ls, ls.ap[:, 2:4], ls, ls.ap[:, 0:2], AF.Exp)
        self.stt(ls, ls.ap[:, 4:5], ls, ls.ap[:, 3:4], -lam_init, ls, ls.ap[:, 2:3], ALU.add, ALU.subtract)
        nlam = ls.ap[:, 4:5]
        r1 = S.alloc("epr1", [512], F32)
        r2 = S.alloc("epr2", [512], F32)
        t1 = S.alloc("ept1", [512], F32)
        t2 = S.alloc("ept2", [512], F32)
        at = S.alloc("epat", [512], F32)
        sq = S.alloc("epsq", [512], BF16)
        rs = S.alloc("eprs", [512], F32)
        hd = [S.alloc("ephd%d" % i, [512], BF16) for i in range(2)]
        ectr = [0]

        def epilogue(h, qc, G, O, Lq):
            self.S.op('vector', [Lq[0]], [r1], lambda e: e.reciprocal(out=r1.ap[:, 0:G], in_=Lq[0].ap[:, 0:G]))
            self.tt('vector', t1, t1.ap[:, 0:G], O[0], O[0].ap[:, 0:G], r1, r1.ap[:, 0:G], ALU.mult)
            self.S.op('vector', [Lq[1]], [r2], lambda e: e.reciprocal(out=r2.ap[:, 0:G], in_=Lq[1].ap[:, 0:G]))
            self.tt('vector', t2, t2.ap[:, 0:G], O[1], O[1].ap[:, 0:G], r2, r2.ap[:, 0:G], ALU.mult)
            self.stt(at, at.ap[:, 0:G], t2, t2.ap[:, 0:G], nlam, t1, t1.ap[:, 0:G], ALU.mult, ALU.add, reads=[ls])
            self.tt('gpsimd', sq, sq.ap[:, 0:G], at, at.ap[:, 0:G], at, at.ap[:, 0:G], ALU.mult)
            zp = Lq[0]
            self.mm(zp, zp.ap[:, 0:G], [self.onesb.ap[:]], [sq.ap[:, 0:G]], [self.onesb, sq])
            self.act(rs, rs.ap[:, 0:G], zp, zp.ap[:, 0:G], AF.Sqrt, reads=[self.epsc], scale=1.0 / 128, bias=self.epsc.ap[:])
            self.S.op('vector', [rs], [rs], lambda e: e.reciprocal(out=rs.ap[:, 0:G], in_=rs.ap[:, 0:G]))
            o = hd[ectr[0] % 2]
            ectr[0] += 1
            self.stt(o, o.ap[:, 0:G], at, at.ap[:, 0:G], 1.0 - lam_init, rs, rs.ap[:, 0:G], ALU.mult, ALU.mult)
            self.store(ot_buf, OT[2 + h, :, qc * 512:qc * 512 + G], o, o.ap[:, 0:G], q='gpsimd')

        def qprov(h):
            qb = qh_b[h % 2]
            self.load(qb, qb.ap[:], QTs[h], src=qt_buf)

            def f(qc):
                G = min(512, nq - qc * 512)
                q0 = qc * 512
                return [[('main', qb, qb.ap[0:64, q0:q0 + G], (0, 64))], [('main', qb, qb.ap[64:128, q0:q0 + G], (64, 128))]]
            return f
        cfg = dict(nh=6, nq=nq, key0=key0, nkeys=nkeys, maps=2, scale=64 ** -0.5,
                   KT=lambda h: self.KT0[h], V=lambda h: self.V0[h], kbuf=self.kt0_buf, vbuf=self.v0_buf,
                   qprov=qprov, epilogue=epilogue)
        self.attention(cfg)
        S.barrier()
        S.release(m0)

    def x_phase(self, tag, layer, which, nq, OT, ot_buf, res_src, res_buf, w_out, H1, h1_buf, A2T, a2t_buf, router=None):
        S = self.S
        m0 = S.mark()
        Wo = S.alloc("xWo", [8, D], BF16)
        self.load(Wo, Wo.ap[:], w_out.rearrange("(c p) n -> p c n", p=128), q='gpsimd')
        G1 = self.load_mod("xG1", layer, which, 2)
        A2 = self.load_mod("xA2", layer, which, 3)
        B2 = self.load_mod("xB2", layer, which, 4)
        t = self.alloc_norm_tmp("x")
        mixg = [S.alloc("xmix%d" % i, [8, 512], BF16) for i in range(2)]
        xg = [S.alloc("xx%d" % i, [4, D], F32) for i in range(2)]
        aT = [S.alloc("xaT%d" % i, [8, 512], BF16) for i in range(2)]
        ytmp = [S.alloc("xyt%d" % i, [512], F32) for i in range(2)]
        PS = self.PS
        if router is not None:
            RWb = S.alloc("xRW", [NEXP, D], F32)
            rw = router['w']
            for e_ in range(NEXP):
                self.load(RWb, RWb.ap[:, e_, :], rw[e_:e_ + 1, :].partition_broadcast(128))
            rj = S.alloc("xrj", [D], F32)
            lg = [S.alloc("xlg%d" % i, [64], F32) for i in range(2)]
        ngr = (nq + 511) // 512
        groups = [(g * 512, min(512, nq - g * 512)) for g in range(ngr)]

        def issue_load(gi):
            t0, G = groups[gi]
            nt = G // 128
            self.load(mixg[gi % 2], mixg[gi % 2].ap[:, :, 0:G], OT[:, :, t0:t0 + G].rearrange("c p n -> p c n"), src=ot_buf)
            self.load(xg[gi % 2], xg[gi % 2].ap[:, 0:nt, :], res_src[t0:t0 + G, :].rearrange("(t p) d -> p t d", p=128), src=res_buf)
        issue_load(0)
        kk = 0
        for gi, (t0, G) in enumerate(groups):
            if gi + 1 < len(groups):
                issue_load(gi + 1)
            nt = G // 128
            mg, xs, aTs = mixg[gi % 2], xg[gi % 2], aT[gi % 2]
            for i in range(nt):
                for hf in range(2):
                    yp = PS[2 + kk % 2]
                    yt = ytmp[kk % 2]
                    kk += 1
                    self.mm(yp, yp.ap[:, :], [mg.ap[:, c, i * 128:(i + 1) * 128] for c in range(8)],
                            [Wo.ap[:, c, hf * 512:(hf + 1) * 512] for c in range(8)], [mg, Wo])
                    self.tt('vector', yt, yt.ap[:], yp, yp.ap[:], G1, G1.ap[:, hf * 512:(hf + 1) * 512], ALU.mult)
                    self.tt('gpsimd', xs, xs.ap[:, i, hf * 512:(hf + 1) * 512], yt, yt.ap[:], xs, xs.ap[:, i, hf * 512:(hf + 1) * 512], ALU.add)
            self.store(h1_buf, H1[t0:t0 + G, :].rearrange("(t p) d -> p t d", p=128), xs, xs.ap[:, 0:nt, :])
            afs = self.run_norm(t, [(xs, xs.ap[:, i, :]) for i in range(nt)], A2, B2,
                                [(aTs, aTs.ap[:, :, i * 128:(i + 1) * 128]) for i in range(nt)],
                                want_f32=(router is not None), tr_banks=(0, 1)) if router is None else None
            if router is not None:
                i = 0
                for af in self.norm_mod_T(t, [(xs, xs.ap[:, i2, :]) for i2 in range(nt)], A2, B2,
                                          [(aTs, aTs.ap[:, :, i2 * 128:(i2 + 1) * 128]) for i2 in range(nt)], want_f32=True):
                    self.router_tile(af, RWb, rj, lg[i % 2], router['wts'], (t0 // 128) + i)
                    i += 1
            self.store(a2t_buf, A2T[:, :, t0:t0 + G].rearrange("c p n -> p c n"), aTs, aTs.ap[:, :, 0:G])
        S.barrier()
        S.release(m0)

    def router_tile(self, af, RWb, rj, lg, wts, tile):
        for e_ in range(NEXP):
            self.stt(rj, rj.ap[:], af, af.ap[:], 1.0, RWb, RWb.ap[:, e_, :], ALU.mult, ALU.mult, writes=[lg], accum_out=lg.ap[:, e_:e_ + 1])
        L_ = lg.ap
        V = 'vector'
        self.S.op(V, [lg], [lg], lambda e: e.tensor_reduce(out=L_[:, 8:9], in_=L_[:, 0:8], axis=AX.X, op=ALU.max))
        self.ts(V, lg, L_[:, 9:10], lg, L_[:, 8:9], -1.0, None, ALU.mult)
        self.ts(V, lg, L_[:, 10:18], lg, L_[:, 0:8], L_[:, 8:9], None, ALU.is_ge)
        self.stt(lg, L_[:, 18:26], lg, L_[:, 10:18], -1e30, lg, L_[:, 0:8], ALU.mult, ALU.add)
        self.S.op(V, [lg], [lg], lambda e: e.tensor_reduce(out=L_[:, 26:27], in_=L_[:, 18:26], axis=AX.X, op=ALU.max))
        self.ts(V, lg, L_[:, 10:18], lg, L_[:, 0:8], L_[:, 26:27], None, ALU.is_ge)
        self.act(lg, L_[:, 27:35], lg, L_[:, 0:8], AF.Exp, bias=L_[:, 9:10], scale=1.0)
        self.tt(V, lg, L_[:, 27:35], lg, L_[:, 27:35], lg, L_[:, 10:18], ALU.mult)
        self.S.op(V, [lg], [lg], lambda e: e.tensor_reduce(out=L_[:, 35:36], in_=L_[:, 27:35], axis=AX.X, op=ALU.add))
        self.S.op(V, [lg], [lg], lambda e: e.reciprocal(out=L_[:, 36:37], in_=L_[:, 35:36]))
        self.ts(V, wts, wts.ap[:, tile, :], lg, L_[:, 27:35], L_[:, 36:37], None, ALU.mult)

    def y_phase(self, tag, layer, which, nq, A2T, a2t_buf, H1, h1_buf, wg, wu, wd, nexp, wts, out_dram, out_buf, final_g=None, T=1024):
        S = self.S
        m0 = S.mark()
        G2 = self.load_mod("yG2", layer, which, 5)
        if final_g is not None:
            FG = S.alloc("yFG", [D], F32)
            self.load(FG, FG.ap[:], final_g.rearrange("(o d) -> o d", o=1).partition_broadcast(128))
        ntmax = T // 128
        aT = S.alloc("yaT", [8, T], BF16)
        acc = S.alloc("yacc", [ntmax, D], F32)
        hT = S.alloc("yhT", [FC, T], BF16)
        NB = 3
        wgb = [S.alloc("ywg%d" % i, [8, 128], BF16) for i in range(NB)]
        wub = [S.alloc("ywu%d" % i, [8, 128], BF16) for i in range(NB)]
        wdh = [S.alloc("ywd%d" % i, [11, D], BF16) for i in range(2)]
        sg = [S.alloc("ysg%d" % i, [512], F32) for i in range(2)]
        yt = [S.alloc("yyt%d" % i, [512], F32) for i in range(2)]
        fss = S.alloc("yfss", [2 * ntmax], F32)
        fjunk = S.alloc("yfj", [D], BF16)
        PS = self.PS
        ngr = (nq + T - 1) // T
        kw = 0
        kd = 0
        kk = 0
        for gi in range(ngr):
            t0 = gi * T
            G = min(T, nq - t0)
            nt = G // 128
            self.load(aT, aT.ap[:, :, 0:G], A2T[:, :, t0:t0 + G].rearrange("c p n -> p c n"), src=a2t_buf)
            self.load(acc, acc.ap[:, 0:nt, :], H1[t0:t0 + G, :].rearrange("(t p) d -> p t d", p=128), src=h1_buf)
            for e_ in range(nexp):
                wge = wg[e_] if nexp > 1 else wg
                wue = wu[e_] if nexp > 1 else wu
                wde = wd[e_] if nexp > 1 else wd
                for j in range(FC):
                    a, b = wgb[kw % NB], wub[kw % NB]
                    kw += 1
                    self.load(a, a.ap[:], wge[:, j * 128:(j + 1) * 128].rearrange("(c p) n -> p c n", p=128), q='gpsimd')
                    self.load(b, b.ap[:], wue[:, j * 128:(j + 1) * 128].rearrange("(c p) n -> p c n", p=128), q='gpsimd')
                    for q0 in range(0, G, 512):
                        Gq = min(512, G - q0)
                        gp, up = PS[kk % 2], PS[2 + kk % 2]
                        s_ = sg[kk % 2]
                        kk += 1
                        rhs = [aT.ap[:, c, q0:q0 + Gq] for c in range(8)]
                        self.mm(gp, gp.ap[:, 0:Gq], [a.ap[:, c, :] for c in range(8)], rhs, [a, aT])
                        self.mm(up, up.ap[:, 0:Gq], [b.ap[:, c, :] for c in range(8)], rhs, [b, aT])
                        self.act(s_, s_.ap[:, 0:Gq], gp, gp.ap[:, 0:Gq], AF.Silu)
                        self.tt('vector', hT, hT.ap[:, j, q0:q0 + Gq], s_, s_.ap[:, 0:Gq], up, up.ap[:, 0:Gq], ALU.mult)
                for hh in range(2):
                    j0, j1 = hh * 11, (hh + 1) * 11
                    self.load(wdh[hh], wdh[hh].ap[:, :, :], wde[j0 * 128:j1 * 128, :].rearrange("(j p) n -> p j n", p=128), q='gpsimd')
                for i in range(nt):
                    dp = [PS[4 + 2 * (kd % 2)], PS[5 + 2 * (kd % 2)]]
                    kd += 1
                    for hf in range(2):
                        for hh in range(2):
                            self.mm(dp[hf], dp[hf].ap[:, :], [hT.ap[:, hh * 11 + jj, i * 128:(i + 1) * 128] for jj in range(11)],
                                    [wdh[hh].ap[:, jj, hf * 512:(hf + 1) * 512] for jj in range(11)],
                                    [hT, wdh[hh]], start=(hh == 0), stop=(hh == 1))
                    for hf in range(2):
                        y_ = yt[hf]
                        if wts is not None:
                            self.stt(y_, y_.ap[:], dp[hf], dp[hf].ap[:], wts.ap[:, t0 // 128 + i, e_:e_ + 1], G2, G2.ap[:, hf * 512:(hf + 1) * 512],
                                     ALU.mult, ALU.mult, reads=[wts])
                        else:
                            self.tt('vector', y_, y_.ap[:], dp[hf], dp[hf].ap[:], G2, G2.ap[:, hf * 512:(hf + 1) * 512], ALU.mult)
                        self.tt('gpsimd', acc, acc.ap[:, i, hf * 512:(hf + 1) * 512], y_, y_.ap[:], acc, acc.ap[:, i, hf * 512:(hf + 1) * 512], ALU.add)
            if final_g is not None:
                for i in range(nt):
                    self.S.op('scalar', [acc], [fjunk, fss],
                              lambda e, i=i: e.activation(out=fjunk.ap[:], in_=acc.ap[:, i, :], func=AF.Square, accum_out=fss.ap[:, i:i + 1]))
                self.act(fss, fss.ap[:, ntmax:ntmax + nt], fss, fss.ap[:, 0:nt], AF.Sqrt, reads=[self.epsc], scale=1.0 / D, bias=self.epsc.ap[:])
                self.S.op('vector', [fss], [fss], lambda e: e.reciprocal(out=fss.ap[:, ntmax:ntmax + nt], in_=fss.ap[:, ntmax:ntmax + nt]))
                for i in range(nt):
                    self.stt(acc, acc.ap[:, i, :], acc, acc.ap[:, i, :], fss.ap[:, ntmax + i:ntmax + i + 1], FG, FG.ap[:], ALU.mult, ALU.mult, reads=[fss])
            self.store(out_buf, out_dram[t0:t0 + G, :].rearrange("(t p) d -> p t d", p=128), acc, acc.ap[:, 0:nt, :])
        S.barrier()
        S.release(m0)


    def l1_latents(self, tag, src, src_buf, n, which, ropeT, CQ, cq_buf, LAT, lat_buf, want_q):
        S = self.S
        m0 = S.mark()
        w_in1 = self.inp_once("odd_w_in", [D, 832])
        qg = self.inp_once("q_norm_g", [512])
        kvg = self.inp_once("kv_norm_g", [256])
        Win = S.alloc("l1Win", [8, 832], BF16)
        self.load(Win, Win.ap[:], w_in1.rearrange("(c p) n -> p c n", p=128), q='gpsimd')
        gq = S.alloc("l1gq", [4], F32)
        gkv = S.alloc("l1gkv", [2], F32)
        self.load(gq, gq.ap[:], qg.rearrange("(c p) -> p c", p=128), allow_slow_non_contiguous=True)
        self.load(gkv, gkv.ap[:], kvg.rearrange("(c p) -> p c", p=128), allow_slow_non_contiguous=True)
        A = self.load_mod("l1A", 1, which, 0)
        B = self.load_mod("l1B", 1, which, 1)
        t = self.alloc_norm_tmp("l1")
        xg = [S.alloc("l1x%d" % i, [4, D], F32) for i in range(2)]
        aT = [S.alloc("l1aT%d" % i, [8, 512], BF16) for i in range(2)]
        cs = [S.alloc("l1cs%d" % i, [2, 512], F32) for i in range(2)]
        zz = [S.alloc("l1z%d" % i, [512], BF16) for i in range(6)]
        sq = [S.alloc("l1sq%d" % i, [512], BF16) for i in range(2)]
        rs = [S.alloc("l1rs%d" % i, [512], F32) for i in range(2)]
        st = [S.alloc("l1st%d" % i, [512], BF16) for i in range(4)]
        kz = S.alloc("l1kz", [512], BF16)
        t1 = S.alloc("l1t1", [512], F32)
        t2 = S.alloc("l1t2", [512], F32)
        PS = self.PS
        ngr = (n + 511) // 512
        groups = [(g * 512, min(512, n - g * 512)) for g in range(ngr)]

        def issue_load(gi):
            t0, G = groups[gi]
            self.load(xg[gi % 2], xg[gi % 2].ap[:, 0:G // 128, :], src[t0:t0 + G, :].rearrange("(t p) d -> p t d", p=128), src=src_buf)
            if ropeT is not None:
                self.load(cs[gi % 2], cs[gi % 2].ap[:, :, 0:G], ropeT[:, :, t0:t0 + G].rearrange("a p n -> p a n"))
        issue_load(0)
        kk = 0
        ks = 0
        for gi, (t0, G) in enumerate(groups):
            if gi + 1 < len(groups):
                issue_load(gi + 1)
            nt = G // 128
            xs, aTs, c_ = xg[gi % 2], aT[gi % 2], cs[gi % 2]
            self.run_norm(t, [(xs, xs.ap[:, i, :]) for i in range(nt)], A, B,
                          [(aTs, aTs.ap[:, :, i * 128:(i + 1) * 128]) for i in range(nt)])
            parts = []
            if want_q:
                parts.append((0, 4, 512.0, gq, CQ, cq_buf, 0, 4))
            parts.append((512, 2, 256.0, gkv, LAT, lat_buf, 4, 5))
            for (c0, nch, dim, gcol, dst, dst_buf, zoff, sbank) in parts:
                ssq = PS[sbank]
                for c in range(nch):
                    zp = PS[2 + kk % 2]
                    kk += 1
                    z = zz[zoff + c]
                    s_ = sq[c % 2]
                    self.mm(zp, zp.ap[:, 0:G], [Win.ap[:, k, c0 + c * 128:c0 + (c + 1) * 128] for k in range(8)],
                            [aTs.ap[:, k, 0:G] for k in range(8)], [Win, aTs])
                    self.cp('scalar', z, z.ap[:, 0:G], zp, zp.ap[:, 0:G])
                    self.act(s_, s_.ap[:, 0:G], zp, zp.ap[:, 0:G], AF.Square)
                    self.mm(ssq, ssq.ap[:, 0:G], [self.onesb.ap[:]], [s_.ap[:, 0:G]], [self.onesb, s_], start=(c == 0), stop=(c == nch - 1))
                r = rs[0 if zoff == 0 else 1]
                self.act(r, r.ap[:, 0:G], ssq, ssq.ap[:, 0:G], AF.Sqrt, reads=[self.epsc], scale=1.0 / dim, bias=self.epsc.ap[:])
                self.S.op('vector', [r], [r], lambda e, r=r, G=G: e.reciprocal(out=r.ap[:, 0:G], in_=r.ap[:, 0:G]))
                for c in range(nch):
                    z = zz[zoff + c]
                    o = st[ks % 4]
                    ks += 1
                    self.stt(o, o.ap[:, 0:G], z, z.ap[:, 0:G], gcol.ap[:, c:c + 1], r, r.ap[:, 0:G], ALU.mult, ALU.mult, reads=[gcol])
                    self.store(dst_buf, dst(c, 0, 128, t0, G), o, o.ap[:, 0:G])
            zp = PS[2 + kk % 2]
            kk += 1
            self.mm(zp, zp.ap[0:64, 0:G], [Win.ap[:, k, 768:832] for k in range(8)], [aTs.ap[:, k, 0:G] for k in range(8)], [Win, aTs])
            o = st[ks % 4]
            ks += 1
            if ropeT is not None:
                self.rope_tile(64, G, zp, kz, PS[6], t1, t2, c_, c_.ap[0:64, 0, 0:G], c_, c_.ap[0:64, 1, 0:G], o, o.ap[0:64, 0:G])
            else:
                self.cp('scalar', o, o.ap[0:64, 0:G], zp, zp.ap[0:64, 0:G])
            self.store(lat_buf, LAT(2, 0, 64, t0, G), o, o.ap[0:64, 0:G])
        S.barrier()
        S.release(m0)

    def l1_kv_build(self, latsrc):
        S = self.S
        m0 = S.mark()
        NK, NT = self.NK, self.NT
        w_ukv = self.inp_once("w_ukv", [256, 2048])
        self.KT1 = self.scratch("KT1", [8, 128, NK], BF16)
        self.V1 = self.scratch("V1", [8, 128, NT, 128], BF16)
        self.kt1_buf = S.dram_buf("KT1")
        self.v1_buf = S.dram_buf("V1")
        Wkn = S.alloc("l1Wkn", [2, 8, 128], BF16, multi=True)
        Wvv = S.alloc("l1Wvv", [2, 8, 128], BF16, multi=True)
        wv = w_ukv.rearrange("(c p) (h x) -> c p h x", p=128, x=256)
        for c in range(2):
            self.load(Wkn, Wkn.ap[:, c, :, :], wv[c, :, :, 0:128], q='gpsimd')
            self.load(Wvv, Wvv.ap[:, c, :, :], wv[c, :, :, 128:256], q='gpsimd')
        latg = [S.alloc("l1lg%d" % i, [2, 512], BF16) for i in range(2)]
        kst = [S.alloc("l1ks%d" % i, [512], BF16) for i in range(4)]
        vst = [S.alloc("l1vs%d" % i, [8, 4, 128], BF16) for i in range(2)]
        PS = self.PS
        groups = [(k0, min(512, NK - k0)) for k0 in range(0, NK, 512)]

        def issue_load(gi):
            k0, G = groups[gi]
            ap_, b_ = latsrc['lat2'](k0, G)
            self.load(latg[gi % 2], latg[gi % 2].ap[:, :, 0:G], ap_, src=b_)
        issue_load(0)
        kk = 0
        for gi, (k0, G) in enumerate(groups):
            if gi + 1 < len(groups):
                issue_load(gi + 1)
            nt = G // 128
            lg = latg[gi % 2]
            for h in range(8):
                zp = PS[kk % 4]
                ks = kst[kk % 4]
                kk += 1
                self.mm(zp, zp.ap[:, 0:G], [Wkn.ap[:, c, h, :] for c in range(2)], [lg.ap[:, c, 0:G] for c in range(2)], [Wkn, lg])
                self.cp('scalar' if h % 2 else 'vector', ks, ks.ap[:, 0:G], zp, zp.ap[:, 0:G])
                self.store(self.kt1_buf, self.KT1[h, :, k0:k0 + G], ks, ks.ap[:, 0:G])
            vs = vst[gi % 2]
            for i in range(nt):
                for hf in range(2):
                    vp = PS[4 + kk % 4]
                    kk += 1
                    self.mm(vp, vp.ap[:, :], [lg.ap[:, c, i * 128:(i + 1) * 128] for c in range(2)],
                            [Wvv.ap[:, c, hf * 4:(hf + 1) * 4, :] for c in range(2)], [Wvv, lg])
                    self.cp('vector' if hf else 'scalar', vs, vs.ap[:, hf * 4:(hf + 1) * 4, i, :], vp, vp.ap[:, :].rearrange("p (h d) -> p h d", h=4))
            tile0 = k0 // 128
            self.store(self.v1_buf, self.V1[:, :, tile0:tile0 + nt, :].rearrange("h p t d -> p h t d"), vs, vs.ap[:, :, 0:nt, :])
        S.barrier()
        S.release(m0)

    def l1_q_build(self, nq, CQ, cq_buf, ropeQ):
        S = self.S
        m0 = S.mark()
        w_uq = self.inp_once("w_uq", [512, 1536])
        self.QN1 = self.scratch("QN1", [8, 128, nq], BF16)
        self.QR1 = self.scratch("QR1", [8, 64, nq], BF16)
        self.qn1_buf = S.dram_buf("QN1")
        self.qr1_buf = S.dram_buf("QR1")
        Wn = S.alloc("l1Wn", [4, 8, 128], BF16, multi=True)
        Wr = S.alloc("l1Wr", [4, 8, 64], BF16, multi=True)
        wv = w_uq.rearrange("(c p) (h x) -> c p h x", p=128, x=192)
        for c in range(4):
            self.load(Wn, Wn.ap[:, c, :, :], wv[c, :, :, 0:128], q='gpsimd')
            self.load(Wr, Wr.ap[:, c, :, :], wv[c, :, :, 128:192], q='gpsimd')
        cqg = [S.alloc("l1cq%d" % i, [4, 512], BF16) for i in range(2)]
        cs = [S.alloc("l1qcs%d" % i, [2, 512], F32) for i in range(2)]
        st = [S.alloc("l1qst%d" % i, [512], BF16) for i in range(4)]
        kz = S.alloc("l1qkz", [512], BF16)
        t1 = S.alloc("l1qt1", [512], F32)
        t2 = S.alloc("l1qt2", [512], F32)
        PS = self.PS
        groups = [(q0, min(512, nq - q0)) for q0 in range(0, nq, 512)]

        def issue_load(gi):
            q0, G = groups[gi]
            self.load(cqg[gi % 2], cqg[gi % 2].ap[:, :, 0:G], CQ[:, :, q0:q0 + G].rearrange("c p n -> p c n"), src=cq_buf)
            self.load(cs[gi % 2], cs[gi % 2].ap[:, :, 0:G], ropeQ[:, :, q0:q0 + G].rearrange("a p n -> p a n"))
        issue_load(0)
        kk = 0
        for gi, (q0, G) in enumerate(groups):
            if gi + 1 < len(groups):
                issue_load(gi + 1)
            cg, c_ = cqg[gi % 2], cs[gi % 2]
            for h in range(8):
                zp = PS[kk % 2]
                zr = PS[2 + kk % 2]
                rp = PS[4 + kk % 2]
                o1 = st[(2 * kk) % 4]
                o2 = st[(2 * kk + 1) % 4]
                kk += 1
                self.mm(zp, zp.ap[:, 0:G], [Wn.ap[:, c, h, :] for c in range(4)], [cg.ap[:, c, 0:G] for c in range(4)], [Wn, cg])
                self.cp('vector', o1, o1.ap[:, 0:G], zp, zp.ap[:, 0:G])
                self.store(self.qn1_buf, self.QN1[h, :, q0:q0 + G], o1, o1.ap[:, 0:G])
                self.mm(zr, zr.ap[0:64, 0:G], [Wr.ap[:, c, h, :] for c in range(4)], [cg.ap[:, c, 0:G] for c in range(4)], [Wr, cg])
                self.rope_tile(64, G, zr, kz, rp, t1, t2, c_, c_.ap[0:64, 0, 0:G], c_, c_.ap[0:64, 1, 0:G], o2, o2.ap[0:64, 0:G])
                self.store(self.qr1_buf, self.QR1[h, :, q0:q0 + G], o2, o2.ap[0:64, 0:G])
        S.barrier()
        S.release(m0)

    def l1_attention(self, nq, latsrc, OT, ot_buf):
        S = self.S
        m0 = S.mark()
        NK = self.NK
        KR = S.alloc("l1KR", [NK], BF16, multi=True)
        for (c0, c1, ap_, b_) in latsrc['kr']:
            self.load(KR, KR.ap[0:64, c0:c1], ap_, src=b_)
        QNb = [S.alloc("l1QN%d" % i, [nq], BF16) for i in range(2)]
        QRb = [S.alloc("l1QR%d" % i, [nq], BF16) for i in range(2)]
        rr = [S.alloc("l1r%d" % i, [512], F32) for i in range(2)]
        oo = [S.alloc("l1o%d" % i, [512], BF16) for i in range(2)]
        ectr = [0]

        def epilogue(h, qc, G, O, Lq):
            r = rr[ectr[0] % 2]
            o = oo[ectr[0] % 2]
            ectr[0] += 1
            self.S.op('vector', [Lq[0]], [r], lambda e: e.reciprocal(out=r.ap[:, 0:G], in_=Lq[0].ap[:, 0:G]))
            self.tt('vector', o, o.ap[:, 0:G], O[0], O[0].ap[:, 0:G], r, r.ap[:, 0:G], ALU.mult)
            self.store(ot_buf, OT[h, :, qc * 512:qc * 512 + G], o, o.ap[:, 0:G], q='gpsimd')

        def qprov(h):
            qn, qr = QNb[h % 2], QRb[h % 2]
            self.load(qn, qn.ap[:], self.QN1[h], src=self.qn1_buf)
            self.load(qr, qr.ap[0:64, :], self.QR1[h], src=self.qr1_buf)

            def f(qc):
                G = min(512, nq - qc * 512)
                q0 = qc * 512
                return [[('main', qn, qn.ap[:, q0:q0 + G], (0, 128)), ('rope', qr, qr.ap[0:64, q0:q0 + G], (0, 64))]]
            return f
        cfg = dict(nh=8, nq=nq, key0=0, nkeys=NK, maps=1, scale=192 ** -0.5,
                   KT=lambda h: self.KT1[h], V=lambda h: self.V1[h], kbuf=self.kt1_buf, vbuf=self.v1_buf,
                   qprov=qprov, epilogue=epilogue, KR=KR)
        self.attention(cfg)
        S.barrier()
        S.release(m0)

    def build_A(self, fused=False):
        S, L, NQ, C = self.S, self.L, self.NQ, self.C
        self.setup_consts()
        self.mod_prologue([0, 1])
        self.l0_kv_build()
        xo = self.inp("xo", [NQ, D])
        xh = self.inp("xh", [128, D])
        xhc = self.inp("xhc", [128, D])
        hmask = self.inp("hmask", [128, 16])
        zmask = self.inp("zmask", [128, 16])
        icnt = self.inp("icnt", [128, 2, NQ])
        icntc = self.inp("icntc", [128, 2, C])
        ropeQ = self.inp("ropeQ", [2, 128, NQ])
        ctxb = self.nc_inputs_ap("ctxb")
        w_out0 = self.inp("even_w_out", [D, D])
        wg0 = self.inp("ffn_w_gate", [D, FF])
        wu0 = self.inp("ffn_w_up", [D, FF])
        wd0 = self.inp("ffn_w_down", [FF, D])
        h2 = self.scratch("h2", [NQ, D], F32) if fused else self.outp("h2", [NQ, D])
        hc2 = self.scratch("hc2", [C, D], F32)
        paths = [
            ("lat", xo, NQ, xh, hmask, 0, ropeQ, icnt, 0, self.NK, h2, "h2"),
            ("ctx", ctxb, C, xhc, zmask, 1, None, icntc, L, C, hc2, "hc2"),
        ]
        for (tag, xq, nq, xhal, hm, which, rq, ic, key0, nkeys, hout, hname) in paths:
            QTs = self.scratch("QTs_" + tag, [6, 128, nq], BF16)
            OT = self.scratch("OT_" + tag, [8, 128, nq], BF16)
            H1 = self.scratch("H1_" + tag, [nq, D], F32)
            A2T = self.scratch("A2T_" + tag, [8, 128, nq], BF16)
            qt_buf, ot_buf = S.dram_buf("QTs_" + tag), S.dram_buf("OT_" + tag)
            h1_buf, a2t_buf = S.dram_buf("H1_" + tag), S.dram_buf("A2T_" + tag)
            mp = S.mark()
            pext = S.alloc("pext", [2, nq + 16], F32)
            self.l0_q_build(tag, xq, nq, xhal, hm, which, rq, QTs, qt_buf, pext)
            self.l0_pool(nq, pext, ic, OT, ot_buf)
            S.barrier()
            S.release(mp)
            self.l0_attention(nq, key0, nkeys, QTs, qt_buf, OT, ot_buf, 0.2)
            self.x_phase(tag, 0, which, nq, OT, ot_buf, xq, None, w_out0, H1, h1_buf, A2T, a2t_buf)
            self.y_phase(tag, 0, which, nq, A2T, a2t_buf, H1, h1_buf, wg0, wu0, wd0, 1, None, hout, S.dram_buf(hname))
        if fused:
            cq = self.scratch("cq", [4, 128, NQ], BF16)
            nlch = NQ // 512
            lat_loc = [self.scratch("lat_loc%d" % j, [384, 512], BF16) for j in range(nlch)]
            latc = self.scratch("latc", [3, 128, C], BF16)
        else:
            cq = self.outp("cq", [4, 128, NQ], BF16)
            lat = self.outp("lat", [3, 128, NQ], BF16)
            latc = self.outp("latc", [3, 128, C], BF16)
        cqf = lambda c, r0, r1, t0, G: cq[c, r0:r1, t0:t0 + G]
        latcf = lambda c, r0, r1, t0, G: latc[c, r0:r1, t0:t0 + G]
        if fused:
            latf = lambda c, r0, r1, t0, G: lat_loc[t0 // 512].rearrange("(c p) n -> c p n", c=3)[c, r0:r1, 0:G]
        else:
            latf = lambda c, r0, r1, t0, G: lat[c, r0:r1, t0:t0 + G]
        self.l1_latents("lat", h2, S.dram_buf("h2"), NQ, 0, ropeQ, cqf, S.dram_buf("cq"), latf, S.dram_buf("lat"), True)
        self.l1_latents("ctx", hc2, S.dram_buf("hc2"), C, 1, None, None, None, latcf, S.dram_buf("latc"), False)
        if not fused:
            return S.finish()
        nqc = L // NQ
        lat_g = [self.scratch("lat_g%d" % j, [nqc * 384, 512], BF16) for j in range(nlch)]
        bg = S.dram_buf("lat_g")
        groups = [list(range(b * nqc, (b + 1) * nqc)) for b in range(2)]
        for j in range(nlch):
            S.dma_custom('gpsimd', lambda e, j=j: e.collective_compute("AllGather", ALU.bypass, replica_groups=groups,
                                                                        ins=[lat_loc[j].opt()], outs=[lat_g[j].opt()]),
                         [S.dram_buf("lat")], [bg], inc=1)
        Gv = [g_.rearrange("(r c p) n -> r c p n", r=nqc, c=3) for g_ in lat_g]
        bc = S.dram_buf("latc")

        def lat2(k0, G):
            if k0 < L:
                r, n0 = k0 // NQ, k0 % NQ
                return Gv[n0 // 512][r, 0:2, :, 0:G].rearrange("c p n -> p c n"), bg
            return latc[0:2, :, k0 - L:k0 - L + G].rearrange("c p n -> p c n"), bc
        kr = [(r * NQ + j * 512, r * NQ + (j + 1) * 512, Gv[j][r, 2, 0:64, :], bg) for r in range(nqc) for j in range(nlch)]
        kr.append((L, L + C, latc[2, 0:64, :], bc))
        latsrc = dict(lat2=lat2, kr=kr)
        w_out1 = self.inp("odd_w_out", [D, D])
        rwT = self.inp("router_wT", [NEXP, D])
        wg1 = self.inp("moe_w_gate", [NEXP, D, FF])
        wu1 = self.inp("moe_w_up", [NEXP, D, FF])
        wd1 = self.inp("moe_w_down", [NEXP, FF, D])
        fg = self.inp("final_norm_g", [D])
        out = self.outp("out", [NQ, D])
        S.rotate()
        self.l1_kv_build(latsrc)
        self.l1_q_build(NQ, cq, S.dram_buf("cq"), ropeQ)
        OT = self.scratch("OT1", [8, 128, NQ], BF16)
        H1 = self.scratch("H1_1", [NQ, D], F32)
        A2T = self.scratch("A2T_1", [8, 128, NQ], BF16)
        ot_buf, h1_buf, a2t_buf = S.dram_buf("OT1"), S.dram_buf("H1_1"), S.dram_buf("A2T_1")
        self.l1_attention(NQ, latsrc, OT, ot_buf)
        mw = S.mark()
        wts = S.alloc("wts", [NQ // 128, NEXP], F32)
        self.x_phase("l1", 1, 0, NQ, OT, ot_buf, h2, S.dram_buf("h2"), w_out1, H1, h1_buf, A2T, a2t_buf, router=dict(w=rwT, wts=wts))
        self.y_phase("l1", 1, 0, NQ, A2T, a2t_buf, H1, h1_buf, wg1, wu1, wd1, NEXP, wts, out, S.dram_buf("out"), final_g=fg)
        S.barrier()
        S.release(mw)
        return S.finish()

    def nc_inputs_ap(self, name):
        return self._inp_aps[name]

    def build_B(self):
        S, L, NQ, C = self.S, self.L, self.NQ, self.C
        self.setup_consts()
        self.mod_prologue([1])
        h2 = self.inp("h2", [NQ, D])
        cq = self.inp("cq", [4, 128, NQ], BF16)
        latall = self.inp("latall", [3, 128, self.NK], BF16)
        ropeQ = self.inp("ropeQ", [2, 128, NQ])
        w_out1 = self.inp("odd_w_out", [D, D])
        rwT = self.inp("router_wT", [NEXP, D])
        wg1 = self.inp("moe_w_gate", [NEXP, D, FF])
        wu1 = self.inp("moe_w_up", [NEXP, D, FF])
        wd1 = self.inp("moe_w_down", [NEXP, FF, D])
        fg = self.inp("final_norm_g", [D])
        out = self.outp("out", [NQ, D])
        latsrc = dict(lat2=lambda k0, G: (latall[0:2, :, k0:k0 + G].rearrange("c p n -> p c n"), None),
                      kr=[(0, self.NK, latall[2, 0:64, :], None)])
        self.l1_kv_build(latsrc)
        self.l1_q_build(NQ, cq, None, ropeQ)
        OT = self.scratch("OT1", [8, 128, NQ], BF16)
        H1 = self.scratch("H1_1", [NQ, D], F32)
        A2T = self.scratch("A2T_1", [8, 128, NQ], BF16)
        ot_buf, h1_buf, a2t_buf = S.dram_buf("OT1"), S.dram_buf("H1_1"), S.dram_buf("A2T_1")
        self.l1_attention(NQ, latsrc, OT, ot_buf)
        mw = S.mark()
        wts = S.alloc("wts", [NQ // 128, NEXP], F32)
        self.x_phase("l1", 1, 0, NQ, OT, ot_buf, h2, None, w_out1, H1, h1_buf, A2T, a2t_buf, router=dict(w=rwT, wts=wts))
        self.y_phase("l1", 1, 0, NQ, A2T, a2t_buf, H1, h1_buf, wg1, wu1, wd1, NEXP, wts, out, S.dram_buf("out"), final_g=fg)
        S.barrier()
        S.release(mw)
        return S.finish()


def _rope_tables(L):
    t = np.arange(L)
    row = (t // GRID_W).astype(np.float32)
    col = (t % GRID_W).astype(np.float32)
    inv = (10000.0 ** (-np.arange(16, dtype=np.float32) / 16)).astype(np.float32)
    ar = row[:, None] * inv[None, :]
    ac = col[:, None] * inv[None, :]
    cos = np.concatenate([np.cos(ar), np.cos(ar), np.cos(ac), np.cos(ac)], axis=-1).astype(np.float32)
    sin = np.concatenate([np.sin(ar), np.sin(ar), np.sin(ac), np.sin(ac)], axis=-1).astype(np.float32)
    cosT = np.concatenate([cos.T, cos.T], axis=0)
    sinT = np.concatenate([sin.T, sin.T], axis=0)
    return np.ascontiguousarray(np.stack([cosT, sinT], axis=0))


def _rmat():
    R = np.zeros((128, 128), np.float32)
    for blk in range(2):
        o = blk * 64
        for i in range(16):
            R[o + 16 + i, o + i] = -1.0
            R[o + i, o + 16 + i] = 1.0
            R[o + 48 + i, o + 32 + i] = -1.0
            R[o + 32 + i, o + 48 + i] = 1.0
    return R


def _icnt(start, n, l):
    out = np.zeros((128, 2, n), np.float32)
    t = np.arange(start, start + n)
    for g, w in enumerate(POOL_WINDOWS):
        lo = np.clip(t - w // 2, 0, l)
        hi = np.clip(t + w // 2, 0, l)
        ic = (1.0 / (hi - lo)).astype(np.float32)
        j, hf = g // 2, g % 2
        out[hf * 64:(hf + 1) * 64, j, :] = ic[None, :]
    return out


_PROG_CACHE = {}


def _get_prog(L, NQ, C, stage):
    key = (L, NQ, C, stage)
    if key not in _PROG_CACHE:
        p = Prog(L, NQ, C, stage)
        p._inp_aps = {}
        orig_inp = p.inp

        def inp(name, shape, dtype=F32):
            ap = orig_inp(name, shape, dtype)
            p._inp_aps[name] = ap
            return ap
        p.inp = inp
        n = p.build_A(fused=True) if stage == 'AB' else (p.build_A() if stage == 'A' else p.build_B())
        p.n_instr = n
        _PROG_CACHE[key] = p
    return _PROG_CACHE[key]


def _run_fused(L, NQ, C, inputs):
    B = inputs["x"].shape[0]
    nqc = L // NQ
    ncores = B * nqc
    f32 = lambda a: np.ascontiguousarray(np.asarray(a, dtype=np.float32))
    x, c, ctx, c_ctx = f32(inputs["x"]), f32(inputs["c"]), f32(inputs["ctx"]), f32(inputs["c_ctx"])
    rope = _rope_tables(L)
    lam = np.stack([f32(inputs["lambda_q1"])[0], f32(inputs["lambda_k1"])[0], f32(inputs["lambda_q2"])[0], f32(inputs["lambda_k2"])[0]], axis=0)
    common = {
        "c_ident": np.eye(128, dtype=np.float32), "c_rmat": _rmat(),
        "mod_w": f32(inputs["mod_w"]), "mod_b": f32(inputs["mod_b"]),
        "norm1_g": f32(inputs["norm1_g"]), "norm2_g": f32(inputs["norm2_g"]),
        "even_w_in": f32(inputs["even_w_in"])[0], "pool_w": f32(inputs["pool_w"])[0], "pool_scale": f32(inputs["pool_scale"])[0],
        "lam": np.ascontiguousarray(lam), "even_w_out": f32(inputs["even_w_out"])[0],
        "ffn_w_gate": f32(inputs["ffn_w_gate"])[0], "ffn_w_up": f32(inputs["ffn_w_up"])[0], "ffn_w_down": f32(inputs["ffn_w_down"])[0],
        "odd_w_in": f32(inputs["odd_w_in"])[0], "q_norm_g": f32(inputs["q_norm_g"])[0], "kv_norm_g": f32(inputs["kv_norm_g"])[0],
        "ropeK": rope, "zmask": np.zeros((128, 16), np.float32), "icntc": _icnt(0, C, C), "xhc": np.zeros((128, D), np.float32),
        "w_uq": f32(inputs["w_uq"])[0], "w_ukv": f32(inputs["w_ukv"])[0], "odd_w_out": f32(inputs["odd_w_out"])[0],
        "router_wT": np.ascontiguousarray(f32(inputs["router_w"])[0].T),
        "moe_w_gate": f32(inputs["moe_w_gate"])[0], "moe_w_up": f32(inputs["moe_w_up"])[0], "moe_w_down": f32(inputs["moe_w_down"])[0],
        "final_norm_g": f32(inputs["final_norm_g"]),
    }
    p = _get_prog(L, NQ, C, 'AB')
    maps = []
    for core in range(ncores):
        b, qc = core // nqc, core % nqc
        s0 = qc * NQ
        cc = np.zeros((128, 8, 2), np.float32)
        cc[:, :, 0] = c[b].reshape(8, 128).T
        cc[:, :, 1] = c_ctx.reshape(8, 128).T
        xh = np.zeros((128, D), np.float32)
        hm = np.zeros((128, 16), np.float32)
        if s0 >= 8:
            xh[0:8] = x[b, s0 - 8:s0]
            hm[:, 0:8] = 1.0
        if s0 + NQ + 8 <= L:
            xh[8:16] = x[b, s0 + NQ:s0 + NQ + 8]
            hm[:, 8:16] = 1.0
        m = dict(common)
        m.update({"cc": cc, "xb": x[b], "ctxb": ctx[b], "xo": np.ascontiguousarray(x[b, s0:s0 + NQ]), "xh": xh, "hmask": hm,
                  "icnt": _icnt(s0, NQ, L), "ropeQ": np.ascontiguousarray(rope[:, :, s0:s0 + NQ])})
        maps.append({k: m[k] for k in p.inputs})
    res = run_bass_kernel_spmd(p.nc, maps, core_ids=list(range(ncores)))
    out = np.zeros((B, L, D), np.float32)
    for core in range(ncores):
        b, qc = core // nqc, core % nqc
        out[b, qc * NQ:(qc + 1) * NQ] = np.asarray(res.results[core]["out"])
    return out


def _run(L, NQ, C, inputs, only_A=False):
    import ml_dtypes
    B = inputs["x"].shape[0]
    nqc = L // NQ
    ncores = B * nqc
    f32 = lambda a: np.ascontiguousarray(np.asarray(a, dtype=np.float32))
    x, c, ctx, c_ctx = f32(inputs["x"]), f32(inputs["c"]), f32(inputs["ctx"]), f32(inputs["c_ctx"])
    rope = _rope_tables(L)
    ident = np.eye(128, dtype=np.float32)
    rmat = _rmat()
    common = {
        "c_ident": ident, "c_rmat": rmat,
        "mod_w": f32(inputs["mod_w"]), "mod_b": f32(inputs["mod_b"]),
        "norm1_g": f32(inputs["norm1_g"]), "norm2_g": f32(inputs["norm2_g"]),
    }
    lam = np.stack([f32(inputs["lambda_q1"])[0], f32(inputs["lambda_k1"])[0], f32(inputs["lambda_q2"])[0], f32(inputs["lambda_k2"])[0]], axis=0)
    wA = {
        "even_w_in": f32(inputs["even_w_in"])[0], "pool_w": f32(inputs["pool_w"])[0], "pool_scale": f32(inputs["pool_scale"])[0],
        "lam": np.ascontiguousarray(lam), "even_w_out": f32(inputs["even_w_out"])[0],
        "ffn_w_gate": f32(inputs["ffn_w_gate"])[0], "ffn_w_up": f32(inputs["ffn_w_up"])[0], "ffn_w_down": f32(inputs["ffn_w_down"])[0],
        "odd_w_in": f32(inputs["odd_w_in"])[0], "q_norm_g": f32(inputs["q_norm_g"])[0], "kv_norm_g": f32(inputs["kv_norm_g"])[0],
        "ropeK": rope, "zmask": np.zeros((128, 16), np.float32), "icntc": _icnt(0, C, C), "xhc": np.zeros((128, D), np.float32),
    }
    wB = {
        "w_uq": f32(inputs["w_uq"])[0], "w_ukv": f32(inputs["w_ukv"])[0], "odd_w_out": f32(inputs["odd_w_out"])[0],
        "router_wT": np.ascontiguousarray(f32(inputs["router_w"])[0].T),
        "moe_w_gate": f32(inputs["moe_w_gate"])[0], "moe_w_up": f32(inputs["moe_w_up"])[0], "moe_w_down": f32(inputs["moe_w_down"])[0],
        "final_norm_g": f32(inputs["final_norm_g"]),
    }
    pA = _get_prog(L, NQ, C, 'A')
    mapsA = []
    for core in range(ncores):
        b, qc = core // nqc, core % nqc
        s0 = qc * NQ
        cc = np.zeros((128, 8, 2), np.float32)
        cc[:, :, 0] = c[b].reshape(8, 128).T
        cc[:, :, 1] = c_ctx.reshape(8, 128).T
        xh = np.zeros((128, D), np.float32)
        hm = np.zeros((128, 16), np.float32)
        if s0 >= 8:
            xh[0:8] = x[b, s0 - 8:s0]
            hm[:, 0:8] = 1.0
        if s0 + NQ + 8 <= L:
            xh[8:16] = x[b, s0 + NQ:s0 + NQ + 8]
            hm[:, 8:16] = 1.0
        m = dict(common)
        m.update(wA)
        m.update({"cc": cc, "xb": x[b], "ctxb": ctx[b], "xo": np.ascontiguousarray(x[b, s0:s0 + NQ]), "xh": xh, "hmask": hm,
                  "icnt": _icnt(s0, NQ, L), "ropeQ": np.ascontiguousarray(rope[:, :, s0:s0 + NQ])})
        mapsA.append({k: m[k] for k in pA.inputs})
    resA = run_bass_kernel_spmd(pA.nc, mapsA, core_ids=list(range(ncores)))
    rA = resA.results
    if only_A:
        return None, rA, None
    pB = _get_prog(L, NQ, C, 'B')
    mapsB = []
    for core in range(ncores):
        b, qc = core // nqc, core % nqc
        s0 = qc * NQ
        cc = np.zeros((128, 8, 2), np.float32)
        cc[:, :, 0] = c[b].reshape(8, 128).T
        cc[:, :, 1] = c_ctx.reshape(8, 128).T
        latall = np.concatenate([np.asarray(rA[b * nqc + j]["lat"]) for j in range(nqc)] + [np.asarray(rA[b * nqc]["latc"])], axis=2)
        m = dict(common)
        m.update(wB)
        m.update({"cc": cc, "h2": np.asarray(rA[core]["h2"]), "cq": np.asarray(rA[core]["cq"]), "latall": np.ascontiguousarray(latall),
                  "ropeQ": np.ascontiguousarray(rope[:, :, s0:s0 + NQ])})
        mapsB.append({k: m[k] for k in pB.inputs})
    resB = run_bass_kernel_spmd(pB.nc, mapsB, core_ids=list(range(ncores)))
    out = np.zeros((B, L, D), np.float32)
    for core in range(ncores):
        b, qc = core // nqc, core % nqc
        out[b, qc * NQ:(qc + 1) * NQ] = np.asarray(resB.results[core]["out"])
    return out, rA, resB.results


def kernel(**inputs):
    L = inputs["x"].shape[1]
    C = inputs["ctx"].shape[1]
    return _run_fused(L, L // 4, C, inputs)
```
